# Optimizing a Trainium2 kernel written in Bass

```python
import jax, jax.numpy as jnp
from jax import lax
import numpy as np

D_MODEL = 1024
BATCH = 16
SEQ = 2048
DEPTH = 2

CHUNK = 64
POOL_WIDTH = 1024
POOL_WINDOWS = (2, 4, 8, 16)
POOL_GROUPS = len(POOL_WINDOWS)
POOL_GROUP_WIDTH = POOL_WIDTH // POOL_GROUPS
HGRN_WIDTH = 1024
HGRN_EXPAND = 128
HGRN_HEADS = HGRN_WIDTH // HGRN_EXPAND
HGRN_HEAD_V = HGRN_WIDTH // HGRN_HEADS
MIX_WIDTH = POOL_WIDTH + HGRN_WIDTH
IN_WIDTH = 2 * POOL_WIDTH + 4 * HGRN_WIDTH
EPS = 1e-6

kernel_name = "hybrid_pool_hgrn2_adaln_block"


def rms_norm(x, w):
    xf = x.astype(jnp.float32)
    xf = xf * lax.rsqrt(jnp.mean(xf * xf, axis=-1, keepdims=True) + EPS)
    return xf * w.astype(jnp.float32)


def multiscale_pool(u, pool_w, pool_scale):
    B, S, _ = u.shape
    cs = jnp.cumsum(u, axis=1)
    count = jnp.arange(1, S + 1, dtype=jnp.float32)[None, :, None]
    outs = []
    for g, w in enumerate(POOL_WINDOWS):
        lo, hi = g * POOL_GROUP_WIDTH, (g + 1) * POOL_GROUP_WIDTH
        cs_g = cs[..., lo:hi]
        prev = jnp.pad(cs_g, ((0, 0), (w, 0), (0, 0)))[:, :S]
        mean = (cs_g - prev) / jnp.minimum(count, float(w))
        outs.append(mean - u[..., lo:hi])
    d = jnp.stack(outs, axis=2)
    y = jnp.einsum('bsgc,gcd->bsgd', d, pool_w.astype(jnp.float32))
    return y.reshape(B, S, POOL_WIDTH) * pool_scale.astype(jnp.float32)


def _to_chunks(t):
    B, S, H, D = t.shape
    return t.reshape(B, S // CHUNK, CHUNK, H, D).transpose(1, 0, 3, 2, 4)


def hgrn2_chunkwise(q, log_f, k, v):
    B, S, H, dk = q.shape
    dv = v.shape[-1]
    causal = jnp.tril(jnp.ones((CHUNK, CHUNK), dtype=bool))[:, :, None]

    def step(state, xs):
        qc, lfc, kc, vc = xs
        b = jnp.cumsum(lfc, axis=2)
        o_inter = jnp.einsum('bhtk,bhkv->bhtv', qc * jnp.exp(b), state)
        diff = b[:, :, :, None, :] - b[:, :, None, :, :]
        decay = jnp.exp(jnp.where(causal, diff, -jnp.inf))
        attn = jnp.einsum('bhtk,bhsk,bhtsk->bhts', qc, kc, decay)
        o_intra = jnp.einsum('bhts,bhsv->bhtv', attn, vc)
        b_last = b[:, :, -1, :]
        k_dec = kc * jnp.exp(b_last[:, :, None, :] - b)
        new_state = jnp.exp(b_last)[..., None] * state + jnp.einsum('bhsk,bhsv->bhkv', k_dec, vc)
        return new_state, o_inter + o_intra

    state0 = jnp.zeros((B, H, dk, dv), jnp.float32)
    _, o = lax.scan(step, state0, (_to_chunks(q), _to_chunks(log_f), _to_chunks(k), _to_chunks(v)))
    return o.transpose(1, 0, 3, 2, 4).reshape(B, S, H, dv)


def setup_inputs(seed: int = 0) -> dict:
    key = jax.random.key(seed)
    ks = jax.random.split(key, 12)
    f32 = jnp.float32
    x = jax.random.normal(ks[0], (BATCH, SEQ, D_MODEL), f32)
    c = jax.random.normal(ks[1], (BATCH, D_MODEL), f32)
    norm_pre_w = 1.0 + 0.05 * jax.random.normal(ks[2], (DEPTH, D_MODEL), f32)
    ada_w = 0.5 * D_MODEL ** -0.5 * jax.random.normal(ks[3], (DEPTH, D_MODEL, 3 * D_MODEL), f32)
    ada_b = 0.01 * jax.random.normal(ks[4], (DEPTH, 3 * D_MODEL), f32)
    w_in = D_MODEL ** -0.5 * jax.random.normal(ks[5], (DEPTH, D_MODEL, IN_WIDTH), f32)
    pool_w = POOL_GROUP_WIDTH ** -0.5 * jax.random.normal(
        ks[6], (DEPTH, POOL_GROUPS, POOL_GROUP_WIDTH, POOL_GROUP_WIDTH), f32)
    pool_scale = 1.0 + 0.05 * jax.random.normal(ks[7], (DEPTH, POOL_WIDTH), f32)
    hgrn_lower_bounds = 0.5 * jax.random.normal(ks[8], (DEPTH, HGRN_WIDTH), f32)
    hgrn_norm_w = 1.0 + 0.05 * jax.random.normal(ks[9], (DEPTH, HGRN_HEAD_V), f32)
    w_out = MIX_WIDTH ** -0.5 * jax.random.normal(ks[10], (DEPTH, MIX_WIDTH, D_MODEL), f32)
    norm_post_w = 1.0 + 0.05 * jax.random.normal(ks[11], (DEPTH, D_MODEL), f32)
    return {"x": x, "c": c, "norm_pre_w": norm_pre_w, "ada_w": ada_w, "ada_b": ada_b,
            "w_in": w_in, "pool_w": pool_w, "pool_scale": pool_scale,
            "hgrn_lower_bounds": hgrn_lower_bounds, "hgrn_norm_w": hgrn_norm_w,
            "w_out": w_out, "norm_post_w": norm_post_w}


def reference(x, c, norm_pre_w, ada_w, ada_b, w_in, pool_w, pool_scale,
              hgrn_lower_bounds, hgrn_norm_w, w_out, norm_post_w):
    B, S, D = x.shape
    f32 = jnp.float32
    lb_sm = jax.nn.softmax(hgrn_lower_bounds.astype(f32), axis=0)
    lower_bounds = jnp.cumsum(lb_sm, axis=0) - lb_sm[0]
    silu_c = jax.nn.silu(c.astype(f32))
    h_res = x.astype(f32)
    for l in range(DEPTH):
        mod = silu_c @ ada_w[l].astype(f32) + ada_b[l].astype(f32)
        shift, scale, gate = jnp.split(mod[:, None, :], 3, axis=-1)
        h = rms_norm(h_res, norm_pre_w[l]) * (1.0 + scale) + shift
        z = h @ w_in[l].astype(f32)
        u_pool, g_pool, q, f_pre, i_in, g_h = jnp.split(
            z, np.cumsum([POOL_WIDTH, POOL_WIDTH, HGRN_WIDTH, HGRN_WIDTH, HGRN_WIDTH]), axis=-1)
        pool_out = multiscale_pool(u_pool, pool_w[l], pool_scale[l]) * jax.nn.silu(g_pool)
        lb = lower_bounds[l]
        f = lb + (1.0 - lb) * jax.nn.sigmoid(f_pre)
        log_f = jnp.log(f)
        k = 1.0 - f
        heads = lambda t, d: t.reshape(B, S, HGRN_HEADS, d)
        o = hgrn2_chunkwise(heads(q, HGRN_EXPAND), heads(log_f, HGRN_EXPAND),
                            heads(k, HGRN_EXPAND), heads(i_in, HGRN_HEAD_V))
        o = rms_norm(o, hgrn_norm_w[l]) * jax.nn.silu(heads(g_h, HGRN_HEAD_V))
        hgrn_out = o.reshape(B, S, HGRN_WIDTH)
        y = jnp.concatenate([pool_out, hgrn_out], axis=-1) @ w_out[l].astype(f32)
        h_res = h_res + gate * rms_norm(y, norm_post_w[l])
    return h_res.astype(x.dtype)
```

```python
import contextlib
import numpy as np
import concourse.bass as bass
import concourse.mybir as mybir
from concourse.bass_utils import run_bass_kernel_spmd

F32 = mybir.dt.float32
BF16 = mybir.dt.bfloat16
AF = mybir.ActivationFunctionType
ALU = mybir.AluOpType

NCORES = 8
B, S, D = 16, 2048, 1024
SEQ_PER_CORE = B // NCORES
DEPTH = 2
T = 512
NG = S // T
NT = T // 128
CH = 32
NCH = T // CH
CPT = 128 // CH
ACLAMP = 5.0e34
EPS = 1e-6
WINDOWS = (2, 4, 8, 16)
NWB = 2
LAT = 450.0
INTERLEAVE = False
ACT_RBF_ALL = True
ENG_LF = "dve"
ENG_T1D = "dve"
KEEP_WARM = False


class Prog:
    def __init__(self):
        self.ops = []
        self.warm = None

    def add(self, eng, fn, reads=(), writes=(), dma=None, cost=300.0, lat=0.0):
        self.ops.append(dict(eng=eng, fn=fn, reads=tuple(reads), writes=tuple(writes), dma=dma,
                             cost=float(cost), lat=float(lat)))

    def _deps(self):
        ops = self.ops
        last_w, readers, last_dma = {}, {}, {}
        for i, op in enumerate(ops):
            op["stream"] = ("dma:" + op["dma"]) if op["dma"] else op["eng"]
            deps = set()
            for r in op["reads"]:
                j = last_w.get(r)
                if j is not None:
                    deps.add(j)
            for w in op["writes"]:
                j = last_w.get(w)
                if j is not None:
                    deps.add(j)
                deps.update(readers.get(w, ()))
            if op["dma"]:
                j = last_dma.get(op["dma"])
                if j is not None:
                    deps.add(j)
                last_dma[op["dma"]] = i
            deps.discard(i)
            for r in op["reads"]:
                readers.setdefault(r, []).append(i)
            for w in op["writes"]:
                last_w[w] = i
                readers[w] = []
            op["deps"] = sorted(deps)

    def _schedule(self, window=24):
        import bisect
        ops = self.ops
        n = len(ops)
        succ = [[] for _ in range(n)]
        rem = [0] * n
        dready = [0.0] * n
        for i, op in enumerate(ops):
            rem[i] = len(op["deps"])
            for j in op["deps"]:
                succ[j].append(i)
        engs = ["pe", "act", "dve", "pool", "sp"]
        ready = {e: [] for e in engs}
        for i, op in enumerate(ops):
            if rem[i] == 0:
                ready[op["eng"]].append(i)
        free = {e: 0.0 for e in engs}
        dma_free = [0.0]
        order = []
        done = 0
        while done < n:
            best = None
            for e in engs:
                rl = ready[e]
                for i in rl[:window]:
                    st = max(free[e], dready[i])
                    key = (st, i)
                    if best is None or key < best[0]:
                        best = (key, i, e)
            (st, _), i, e = best
            ready[e].remove(i)
            op = ops[i]
            op["gap"] = st - free[e]
            free[e] = st + op["cost"]
            op["t0"] = st
            if op["dma"]:
                t0_ = max(st + op["cost"], dma_free[0])
                dma_free[0] = t0_ + op["lat"]
                fin = dma_free[0] + 2000.0
            else:
                fin = st + op["cost"] + op["lat"]
            order.append(i)
            done += 1
            for s_ in succ[i]:
                if fin > dready[s_]:
                    dready[s_] = fin
                rem[s_] -= 1
                if rem[s_] == 0:
                    bisect.insort(ready[ops[s_]["eng"]], s_)
        self.makespan = max(free.values())
        return order

    def finalize(self):
        self._deps()
        order = self._schedule()
        ops = self.ops
        pos = [0] * len(ops)
        for k_, i in enumerate(order):
            pos[i] = k_
        for i, op in enumerate(ops):
            op["signal"] = bool(op["dma"])
        for i, op in enumerate(ops):
            latest = {}
            for j in op["deps"]:
                dj = ops[j]
                if dj["stream"] == op["stream"] and op["eng"] == "pe" and not op["dma"]:
                    continue
                st = dj["stream"]
                if st not in latest or pos[j] > pos[latest[st]]:
                    latest[st] = j
            op["sdeps"] = list(latest.values())
            for j in op["sdeps"]:
                ops[j]["signal"] = True
        tick = {}
        for i in order:
            op = ops[i]
            if op["signal"] and op["fn"] is not None:
                st = op["stream"]
                tick[st] = tick.get(st, 0) + (16 if op["dma"] else 1)
                op["tick"] = tick[st]
        waited = {}
        for i in order:
            op = ops[i]
            need = {}
            for j in op["sdeps"]:
                dj = ops[j]
                need[dj["stream"]] = max(need.get(dj["stream"], 0), dj["tick"])
            w = []
            for st, v in need.items():
                key = (op["eng"], st)
                if waited.get(key, 0) < v:
                    waited[key] = v
                    w.append((st, v))
            op["waits"] = w
        self.order = order
        return sorted(tick.keys())

    def emit(self, nc, stack):
        streams = self.finalize()
        sems = {st: stack.enter_context(nc.semaphore("s_" + st.replace(":", "_"))) for st in streams}
        block = stack.enter_context(nc.Block())
        ops = self.ops
        order = self.order

        def run(engname, e):
            for i in order:
                op = ops[i]
                if op["eng"] != engname:
                    continue
                if engname == "pe" and self.warm is not None and op.get("gap", 0) > 1500.0 and op["waits"]:
                    for _ in range(min(int(0.6 * op["gap"] / 70.0), 96)):
                        e.ldweights(self.warm)
                for st, v in op["waits"]:
                    e.wait_ge(sems[st], v)
                if op["fn"] is None:
                    continue
                inst = op["fn"](e)
                if op["signal"]:
                    inst.then_inc(sems[op["stream"]], 16 if op["dma"] else 1)

        @block.sync
        def _(e):
            run("sp", e)

        @block.gpsimd
        def _(e):
            run("pool", e)

        @block.scalar
        def _(e):
            run("act", e)

        @block.vector
        def _(e):
            run("dve", e)

        @block.tensor
        def _(e):
            run("pe", e)


def _band_mats():
    m = np.zeros((128, 12, 128), np.float32)
    for g, w in enumerate(WINDOWS):
        for tp in range(128):
            for t in range(max(0, tp - w + 1), tp + 1):
                m[t, g, tp] += 1.0 / w
            m[tp, g, tp] -= 1.0
            for t in range(128):
                if t - 128 > tp - w:
                    m[t, 4 + g, tp] += 1.0 / w
            cnt = min(tp + 1, w)
            for t in range(max(0, tp - w + 1), tp + 1):
                m[t, 8 + g, tp] += 1.0 / cnt
            m[tp, 8 + g, tp] -= 1.0
    return m


def _consts():
    mask_scan = np.ones((128, T), np.float32)
    mask_scan[:, ::CH] = 0.0
    bd = np.zeros((128, 128), np.float32)
    for s_ in range(128):
        for t_ in range(128):
            if s_ // CH == t_ // CH and s_ <= t_:
                bd[s_, t_] = 1.0
    mask_bd = np.tile(bd, (1, NT))
    cb = np.zeros((128, 3, 128), np.float32)
    cb[:, 0, :] = np.eye(128, dtype=np.float32)
    cb[:, 1, :] = 1.0 / D
    cb[:, 2, :] = 1.0 / 128
    return mask_scan, mask_bd, cb, _band_mats()


def build(n_seq=SEQ_PER_CORE, n_groups=NG, depth=DEPTH, debug=False, stop=None):
    nc = bass.Bass("TRN2", target_bir_lowering=False)
    P = Prog()
    stack = contextlib.ExitStack()

    def dram(name, shape, kind="ExternalInput"):
        return nc.dram_tensor(name, list(shape), F32, kind=kind).ap()

    xT_d = dram("xT", [SEQ_PER_CORE, 128, 8, S])
    cT_d = dram("cT", [128, 8, SEQ_PER_CORE])
    adaw_d = dram("adaw", [DEPTH, 6, 128, 8, 512])
    adab_d = dram("adab", [128, DEPTH, 24])
    wpg_d = dram("wpg", [DEPTH, 6, 128, 8, 512])
    whq_d = dram("whq", [DEPTH, 8, 128, 8, 384])
    poolw_d = dram("poolw", [128, DEPTH, 4, 2, 256])
    wout_d = dram("wout", [DEPTH, 128, 16, 1024])
    npre_d = dram("npre", [128, DEPTH, 8])
    pscale_d = dram("pscale", [128, DEPTH, 8])
    npost_d = dram("npost", [128, DEPTH, 8])
    hnw_d = dram("hnw", [128, DEPTH])
    lbraw_d = dram("lbraw", [128, DEPTH, 8])
    mscan_d = dram("mscan", [128, T])
    mbd_d = dram("mbd", [128, T])
    cb_d = dram("cb", [128, 3, 128])
    band_d = dram("band", [128, 12, 128])
    outT_d = dram("outT", [SEQ_PER_CORE, 128, 8, S], kind="ExternalOutput")
    dbg_d = {}

    def sb(name, shape, dt=F32):
        return stack.enter_context(nc.sbuf_tensor(name, list(shape), dt))

    ps = [stack.enter_context(nc.psum_tensor("ps%d" % i, [128, 512], F32)) for i in range(8)]
    PS = ["ps%d" % i for i in range(8)]

    xg = [sb("xg%d" % i, [128, 8, T]) for i in range(2)]
    hT = sb("hT", [128, 8, T], BF16)
    mixT = sb("mixT", [128, 16, T], BF16)
    sgw = sb("sgw", [128, 8, T], BF16)
    wbuf = [sb("wbuf%d" % i, [128, 8, 512], BF16) for i in range(NWB)]
    woutb = sb("woutb", [128, 16, 1024], BF16)
    poolwb = sb("poolwb", [128, 4, 2, 256], BF16)
    Sst = [sb("Sst%d" % l, [128, 8, 128]) for l in range(DEPTH)]
    uprev = [sb("uprev%d" % l, [128, 4, 256], BF16) for l in range(DEPTH)]
    mscan = sb("mscan_s", [128, T])
    mbd = sb("mbd_s", [128, T])
    cbb = sb("cbb", [128, 3, 128], BF16)
    bandb = sb("bandb", [128, 12, 128], BF16)
    cT = sb("cT_s", [128, 8, SEQ_PER_CORE])
    scT = sb("scT", [128, 8, SEQ_PER_CORE], BF16)
    adab = sb("adab_s", [128, DEPTH, 24])
    modT = sb("modT", [128, DEPTH, 24, SEQ_PER_CORE])
    npre = sb("npre_s", [128, DEPTH, 8])
    pscale = sb("pscale_s", [128, DEPTH, 8])
    npost = sb("npost_s", [128, DEPTH, 8])
    hnw = sb("hnw_s", [128, DEPTH])
    lbraw = sb("lbraw_s", [128, DEPTH, 8])
    lbe = sb("lbe", [128, DEPTH, 8])
    lbs = sb("lbs", [128, 8])
    lbr = sb("lbr", [128, 8])
    lbsm = sb("lbsm", [128, DEPTH, 8])
    lb = sb("lb", [128, DEPTH, 8])
    ln1mlb = sb("ln1mlb", [128, DEPTH, 8])
    Acoef = sb("Acoef", [128, DEPTH, SEQ_PER_CORE, 8])
    SHcoef = sb("SHcoef", [128, DEPTH, SEQ_PER_CORE, 8])
    GWcoef = sb("GWcoef", [128, DEPTH, SEQ_PER_CORE, 8])
    utok = [sb("utok%d" % i, [128, NT, 256], BF16) for i in range(2)]
    dT = [sb("dT%d" % i, [128, 2, T], BF16) for i in range(2)]
    sgp = [sb("sgp%d" % i, [128, 2, T], BF16) for i in range(2)]
    HS = []
    for i in range(2):
        HS.append(dict(
            e_t=sb("e_t%d" % i, [128, T]),
            L1=sb("L1%d" % i, [128, T]),
            L2=sb("L2%d" % i, [128, T]),
            bcum=sb("bcum%d" % i, [128, T]),
            Dm=sb("Dm%d" % i, [128, T]),
            dec=sb("dec%d" % i, [128, NCH]),
            QT=sb("QT%d" % i, [128, T], BF16),
            KT=sb("KT%d" % i, [128, T], BF16),
            vtok=sb("vtok%d" % i, [128, NT, 128], BF16),
            vblk=sb("vblk%d" % i, [128, NT, CPT, 128], BF16),
            ktok=sb("ktok%d" % i, [128, NT, 128], BF16),
            attnm=sb("attnm%d" % i, [128, T], BF16),
            Rbf=sb("Rbf%d" % i, [128, NCH, 128], BF16),
            Sall=sb("Sall%d" % i, [128, 9, 128]),
        ))
    lnv, rstd = HS[0]["bcum"], HS[1]["bcum"]
    tA = [HS[0]["Dm"], HS[1]["Dm"]]
    tO = [HS[0]["L1"], HS[1]["L1"]]

    ident = cbb[:, 0, :]
    if KEEP_WARM:
        P.warm = cbb[:, 0, :]
    onesD = cbb[:, 1, :]
    onesV = cbb[:, 2, :]

    def fsz(ap):
        n = 1
        for d_ in ap.shape[1:]:
            n *= int(d_)
        return n

    def dma(eng, key, out, in_, reads, writes, nbytes=0):
        P.add(eng, lambda e: e.dma_start(out=out, in_=in_), reads, writes, dma=key,
              cost=(1500.0 if eng == "pool" else 300.0), lat=nbytes / 190.0)

    def mm(out, lhsT, rhs, start, stop, reads, writes):
        P.add("pe", lambda e: e.matmul(out, lhsT=lhsT, rhs=rhs, start=start, stop=stop), reads, writes,
              cost=max(fsz(rhs), 64, min(fsz(lhsT), 128) * 0.6) / 1.7 + 8.0, lat=LAT)

    def tr(out, in_, reads, writes):
        P.add("pe", lambda e: e.transpose(out, in_, ident), reads, writes, cost=80.0, lat=LAT)

    def act(out, in_, func, reads, writes, scale=None, bias=None):
        kw = {}
        if scale is not None:
            kw["scale"] = scale
        if bias is not None:
            kw["bias"] = bias
        P.add("act", lambda e: e.activation(out=out, in_=in_, func=func, **kw), reads, writes,
              cost=220.0 + fsz(in_) / 1.3, lat=LAT)

    def tt(out, in0, in1, op, reads, writes, eng="dve"):
        c = 80.0 + fsz(in0) * 1.6 if eng == "dve" else 200.0 + fsz(in0) * 3.0
        P.add(eng, lambda e: e.tensor_tensor(out=out, in0=in0, in1=in1, op=op), reads, writes, cost=c, lat=LAT)

    def ts(out, in0, s1, op0, reads, writes, s2=None, op1=None, eng="dve"):
        c = 80.0 + fsz(in0) * 1.1 if eng == "dve" else 200.0 + fsz(in0) * 2.5
        if op1 is None:
            P.add(eng, lambda e: e.tensor_scalar(out=out, in0=in0, scalar1=s1, scalar2=None, op0=op0), reads, writes,
                  cost=c, lat=LAT)
        else:
            P.add(eng, lambda e: e.tensor_scalar(out=out, in0=in0, scalar1=s1, scalar2=s2, op0=op0, op1=op1),
                  reads, writes, cost=c, lat=LAT)

    def stt(out, in0, scalar, in1, op0, op1, reads, writes):
        P.add("dve", lambda e: e.scalar_tensor_tensor(out=out, in0=in0, scalar=scalar, in1=in1, op0=op0, op1=op1),
              reads, writes, cost=120.0 + fsz(in0) * 1.6, lat=LAT)

    def cp(eng, out, in_, reads, writes):
        c = 80.0 + fsz(in_) * 1.1 if eng == "dve" else 200.0 + fsz(in_) * 2.5
        P.add(eng, lambda e: e.tensor_copy(out=out, in_=in_), reads, writes, cost=c, lat=LAT)

    dma("sp", "c0", cT[:], cT_d, [], ["cT"])
    dma("sp", "c1", adab[:], adab_d, [], ["adab"])
    dma("sp", "c2", npre[:], npre_d, [], ["npre"])
    dma("sp", "c3", pscale[:], pscale_d, [], ["pscale"])
    dma("sp", "c4", npost[:], npost_d, [], ["npost"])
    dma("sp", "c5", hnw[:], hnw_d, [], ["hnw"])
    dma("sp", "c6", lbraw[:], lbraw_d, [], ["lbraw"])
    dma("sp", "c7", mscan[:], mscan_d, [], ["mscan"])
    dma("sp", "c8", mbd[:], mbd_d, [], ["mbd"])
    dma("pool", "c9", cbb[:], cb_d, [], ["cbb"])
    dma("pool", "c10", bandb[:], band_d, [], ["bandb"])

    wcount = [0]

    def load_w(src, ncols):
        i = wcount[0] % NWB
        wcount[0] += 1
        dma("pool", "w%d" % i, wbuf[i][:, :, 0:ncols], src, [], ["wbuf%d" % i], nbytes=128 * 8 * ncols * 4)
        return wbuf[i], "wbuf%d" % i

    for i_ in range(2):
        P.add("dve", (lambda i_=i_: lambda e: e.memset(HS[i_]["vblk"][:], 0.0))(), [], ["vblk%d" % i_])
    act(scT[:], cT[:], AF.Silu, ["cT"], ["scT"])
    def ada_layer(l):
        for blk in range(6):
            wb, wn = load_w(adaw_d[l, blk], 512)
            for j in range(4):
                col = blk * 4 + j
                o = ps[0][:, (l * 24 + col) * SEQ_PER_CORE:(l * 24 + col + 1) * SEQ_PER_CORE]
                for kc in range(8):
                    mm(o, wb[:, kc, j * 128:(j + 1) * 128], scT[:, kc, :], kc == 0, kc == 7,
                       [wn, "scT"], [PS[0]])
        tt(modT[:, l], ps[0][:, l * 24 * SEQ_PER_CORE:(l + 1) * 24 * SEQ_PER_CORE].rearrange(
            "p (c b) -> p c b", b=SEQ_PER_CORE),
           adab[:, l, :].unsqueeze(2).to_broadcast([128, 24, SEQ_PER_CORE]), ALU.add,
           [PS[0], "adab"], ["modT%d" % l])
        for b in range(SEQ_PER_CORE):
            stt(Acoef[:, l, b, :], modT[:, l, 8:16, b], 1.0, npre[:, l, :], ALU.add, ALU.mult,
                ["modT%d" % l, "npre"], ["Acoef%d" % l])
            P.add("dve", (lambda l=l, b=b: lambda e: e.tensor_copy(out=SHcoef[:, l, b, :], in_=modT[:, l, 0:8, b]))(),
                  ["modT%d" % l], ["SHcoef%d" % l])
            tt(GWcoef[:, l, b, :], modT[:, l, 16:24, b], npost[:, l, :], ALU.mult, ["modT%d" % l, "npost"],
               ["GWcoef%d" % l])

    ada_layer(0)
    act(lbe[:], lbraw[:], AF.Exp, ["lbraw"], ["lbe"])
    tt(lbs[:], lbe[:, 0, :], lbe[:, 1, :], ALU.add, ["lbe"], ["lbs"])
    P.add("dve", lambda e: e.reciprocal(out=lbr[:], in_=lbs[:]), ["lbs"], ["lbr"])
    for l in range(DEPTH):
        tt(lbsm[:, l, :], lbe[:, l, :], lbr[:], ALU.mult, ["lbe", "lbr"], ["lbsm"])
    tt(lb[:, 0, :], lbsm[:, 0, :], lbsm[:, 0, :], ALU.subtract, ["lbsm"], ["lb0"])
    tt(lbs[:], lbsm[:, 0, :], lbsm[:, 1, :], ALU.add, ["lbsm", "lbr"], ["lbs2"])
    tt(lb[:, 1, :], lbs[:], lbsm[:, 0, :], ALU.subtract, ["lbs2", "lbsm"], ["lb1"])
    act(ln1mlb[:], lb[:], AF.Ln, ["lb0", "lb1"], ["ln1mlb"], scale=-1.0, bias=1.0)
    LBR = ["lb0", "lb1", "ln1mlb"]

    unit = [0]
    marks = []
    P.marks = marks

    def next_set():
        u = unit[0] % 2
        unit[0] += 1
        return u, 4 * u

    def layer_pass(l, b, G, xt, xn):
        first = (G == 0)
        marks.append(('A l%d b%d G%d' % (l, b, G), len(P.ops)))
        dma("pool", "wo", woutb[:], wout_d[l], [], ["woutb"], nbytes=8 << 20)
        dma("pool", "pw", poolwb[:], poolw_d[:, l], [], ["poolwb"], nbytes=512 << 10)
        u, pb = next_set()
        sqA = mixT[:, 0:8, :]
        for k2 in range(4):
            act(mixT[:, 2 * k2:2 * k2 + 2, :], xt[:, 2 * k2:2 * k2 + 2, :], AF.Square, [xn], ["mixT"])
            for kc in (2 * k2, 2 * k2 + 1):
                mm(ps[pb + 3][:], onesD, mixT[:, kc, :], kc == 0, kc == 7, ["mixT", "cbb"], [PS[pb + 3]])
        act(lnv[:], ps[pb + 3][:], AF.Ln, [PS[pb + 3]], ["bcum0"], bias=EPS)
        act(rstd[:], lnv[:], AF.Exp, ["bcum0"], ["bcum1"], scale=-0.5)
        for kc in range(8):
            tb = tA[kc % 2]
            tn = "Dm%d" % (kc % 2)
            tt(tb[:], xt[:, kc, :], rstd[:], ALU.mult, [xn, "bcum1"], [tn], eng=("dve" if kc % 4 != 3 else "pool"))
            act(hT[:, kc, :], tb[:], AF.Identity, [tn, "Acoef%d" % l, "SHcoef%d" % l], ["hT"],
                scale=Acoef[:, l, b, kc:kc + 1], bias=SHcoef[:, l, b, kc:kc + 1])
        if stop == 'A':
            return
        def unit_P(g):
            marks.append(('P%d l%d' % (g, l), len(P.ops)))
            u, pb = next_set()
            wb, wn = load_w(wpg_d[l, g], 512)
            un, dn, sn_ = "utok%d" % u, "dT%d" % u, "sgp%d" % u
            for oc in range(2):
                for kc in range(8):
                    mm(ps[pb + oc][:], wb[:, kc, 256 + oc * 128:256 + (oc + 1) * 128], hT[:, kc, :], kc == 0, kc == 7,
                       [wn, "hT"], [PS[pb + oc]])
            for j in range(NT):
                o = ps[pb + 2 + j // 2][:, (j % 2) * 256:(j % 2 + 1) * 256]
                for kc in range(8):
                    mm(o, hT[:, kc, j * 128:(j + 1) * 128], wb[:, kc, 0:256], kc == 0, kc == 7,
                       [wn, "hT"], [PS[pb + 2 + j // 2]])
            for oc in range(2):
                act(sgp[u][:, oc, :], ps[pb + oc][:], AF.Silu, [PS[pb + oc]], [sn_])
            for jj in range(NT // 2):
                cp("dve", utok[u][:, 2 * jj:2 * jj + 2, :], ps[pb + 2 + jj][:].rearrange("p (j c) -> p j c", c=256),
                   [PS[pb + 2 + jj]], [un])
            for cc in range(2):
                bk = pb + 2 + cc
                for j in range(NT):
                    o = ps[bk][:, j * 128:(j + 1) * 128]
                    if j == 0:
                        if first:
                            mm(o, utok[u][:, 0, cc * 128:(cc + 1) * 128], bandb[:, 8 + g, :], True, True,
                               [un, "bandb"], [PS[bk]])
                        else:
                            mm(o, utok[u][:, 0, cc * 128:(cc + 1) * 128], bandb[:, g, :], True, False,
                               [un, "bandb"], [PS[bk]])
                            mm(o, uprev[l][:, g, cc * 128:(cc + 1) * 128], bandb[:, 4 + g, :], False, True,
                               ["uprev%d_%d" % (l, g), "bandb"], [PS[bk]])
                    else:
                        mm(o, utok[u][:, j, cc * 128:(cc + 1) * 128], bandb[:, g, :], True, False,
                           [un, "bandb"], [PS[bk]])
                        mm(o, utok[u][:, j - 1, cc * 128:(cc + 1) * 128], bandb[:, 4 + g, :], False, True,
                           [un, "bandb"], [PS[bk]])
            cp("pool", uprev[l][:, g, :], utok[u][:, NT - 1, :], [un], ["uprev%d_%d" % (l, g)])
            for cc in range(2):
                act(dT[u][:, cc, :], ps[pb + 2 + cc][:], AF.Copy, [PS[pb + 2 + cc]], [dn])
            for oc in range(2):
                for cc in range(2):
                    mm(ps[pb + oc][:], poolwb[:, g, cc, oc * 128:(oc + 1) * 128], dT[u][:, cc, :], cc == 0, cc == 1,
                       ["poolwb", dn], [PS[pb + oc]])
            for oc in range(2):
                stt(mixT[:, 2 * g + oc, :], ps[pb + oc][:], pscale[:, l, 2 * g + oc:2 * g + oc + 1], sgp[u][:, oc, :],
                    ALU.mult, ALU.mult, [PS[pb + oc], "pscale", sn_], ["mixT"])
        def unit_G(hb):
            marks.append(('G%d l%d' % (hb, l), len(P.ops)))
            u, pb = next_set()
            wb, wn = load_w(wpg_d[l, 4 + hb], 512)
            for oc in range(4):
                for kc in range(8):
                    mm(ps[pb + oc][:], wb[:, kc, oc * 128:(oc + 1) * 128], hT[:, kc, :], kc == 0, kc == 7,
                       [wn, "hT"], [PS[pb + oc]])
            for oc in range(4):
                act(sgw[:, hb * 4 + oc, :], ps[pb + oc][:], AF.Silu, [PS[pb + oc]], ["sgw%d" % (hb * 4 + oc)])
        def unit_H(h):
            marks.append(('H%d l%d' % (h, l), len(P.ops)))
            u, pb = next_set()
            pq, pf, pv, pa = pb, pb + 1, pb + 2, pb + 3
            X = HS[u]
            e_t, L1, L2, bcum, Dm, dec = X["e_t"], X["L1"], X["L2"], X["bcum"], X["Dm"], X["dec"]
            QT, KT, vtok, vblk, ktok, attnm, Rbf, SA, sqH = (X["QT"], X["KT"], X["vtok"], X["vblk"], X["ktok"],
                                                           X["attnm"], X["Rbf"], X["Sall"], X["attnm"])

            def R(nm, u=u):
                return "%s%d" % (nm, u)

            wb, wn = load_w(whq_d[l, h], 384)
            for kc in range(8):
                mm(ps[pq][:], wb[:, kc, 0:128], hT[:, kc, :], kc == 0, kc == 7, [wn, "hT"], [PS[pq]])
            for kc in range(8):
                mm(ps[pf][:], wb[:, kc, 128:256], hT[:, kc, :], kc == 0, kc == 7, [wn, "hT"], [PS[pf]])
            for j in range(NT):
                for kc in range(8):
                    mm(ps[pv][:, j * 128:(j + 1) * 128], hT[:, kc, j * 128:(j + 1) * 128], wb[:, kc, 256:384],
                       kc == 0, kc == 7, [wn, "hT"], [PS[pv]])
            act(e_t[:], ps[pf][:], AF.Exp, [PS[pf]], [R("e_t")], scale=-1.0)
            act(L2[:], e_t[:], AF.Ln, [R("e_t")], [R("L2")], bias=1.0)
            act(L1[:], e_t[:], AF.Ln, [R("e_t")] + LBR, [R("L1")], scale=lb[:, l, h:h + 1], bias=1.0)
            act(vtok[:], ps[pv][:].rearrange("p (j c) -> p j c", c=128), AF.Copy, [PS[pv]], [R("vtok")])
            for q in range(CPT):
                act(vblk[q * CH:(q + 1) * CH, :, q, :], ps[pv][q * CH:(q + 1) * CH, :].rearrange("p (j c) -> p j c", c=128),
                    AF.Copy, [PS[pv]], [R("vblk")])
            tt(L1[:], L1[:], L2[:], ALU.subtract, [R("L1"), R("L2")], [R("L1")], eng=ENG_LF)
            P.add("dve", (lambda bcum=bcum, L1=L1: lambda e: e.tensor_tensor_scan(
                out=bcum[:], data0=mscan[:], data1=L1[:], initial=0.0, op0=ALU.mult, op1=ALU.add))(),
                [R("L1"), "mscan"], [R("bcum")], cost=80.0 + T * 2.1, lat=LAT)
            b3 = bcum[:].rearrange("p (c t) -> p c t", t=CH)
            tt(Dm[:].rearrange("p (c t) -> p c t", t=CH), b3, b3[:, :, CH - 1:CH].to_broadcast([128, NCH, CH]),
               ALU.subtract, [R("bcum")], [R("Dm")])
            act(dec[:], b3[:, :, CH - 1], AF.Exp, [R("bcum")], [R("dec")])
            act(e_t[:], Dm[:], AF.Exp, [R("Dm")], [R("e_t")])
            stt(QT[:], e_t[:], ACLAMP, ps[pq][:], ALU.min, ALU.mult, [PS[pq], R("e_t")], [R("QT")])
            tt(L2[:], ps[pf][:], L2[:], ALU.add, [PS[pf], R("L2")], [R("L2")])
            tt(L2[:], L2[:], Dm[:], ALU.add, [R("L2"), R("Dm")], [R("L2")], eng=ENG_T1D)
            act(KT[:], L2[:], AF.Exp, [R("L2")] + LBR, [R("KT")], scale=-1.0, bias=ln1mlb[:, l, h:h + 1])
            ktp = ps[pv][:].bitcast(BF16)
            for j in range(NT):
                tr(ktp[:, j * 128:(j + 1) * 128], KT[:, j * 128:(j + 1) * 128], [R("KT"), "cbb"], [PS[pv]])
            act(ktok[:].rearrange("p j c -> p (j c)"), ktp[:, 0:T], AF.Copy, [PS[pv]], [R("ktok")])
            for j in range(NT):
                mm(ps[pa][:, j * 128:(j + 1) * 128], KT[:, j * 128:(j + 1) * 128], QT[:, j * 128:(j + 1) * 128],
                   True, True, [R("KT"), R("QT")], [PS[pa]])
            tt(attnm[:], ps[pa][:], mbd[:], ALU.mult, [PS[pa], "mbd"], [R("attnm")])
            sn = "S%d_%d" % (l, h)

            def SL(k_):
                return "%s_%d" % (R("Sall"), k_)

            for hh in range(2):
                if hh == 0:
                    cp("pool", SA[:, 0, :], Sst[l][:, h, :], [sn], [SL(0)])
                else:
                    cp("pool", SA[:, 0, :], SA[:, 8, :], [SL(8)], [SL(0)])
                for jj in range(2):
                    j = 2 * hh + jj
                    bank = (pq, pf)[jj]
                    mm(ps[bank][:], ktok[:, j, :], vblk[:, j, :, :].rearrange("p q c -> p (q c)"), True, True,
                       [R("ktok"), R("vblk")], [PS[bank]])
                for i_ in range(8):
                    c = 8 * hh + i_
                    bank = (pq, pf)[i_ // 4]
                    if hh == 1 or ACT_RBF_ALL:
                        act(Rbf[:, c, :], SA[:, i_, :], AF.Identity, [SL(i_), R("dec")], [R("Rbf") + "_%d" % hh],
                            scale=dec[:, c:c + 1])
                    stt(SA[:, i_ + 1, :], SA[:, i_, :], dec[:, c:c + 1], ps[bank][:, (i_ % 4) * 128:(i_ % 4 + 1) * 128],
                        ALU.mult, ALU.add, [SL(i_), R("dec"), PS[bank]], [SL(i_ + 1)])
                if hh == 0 and not ACT_RBF_ALL:
                    tt(Rbf[:, 0:8, :], SA[:, 0:8, :],
                       dec[:, 0:8].unsqueeze(2).to_broadcast([128, 8, 128]), ALU.mult,
                       [SL(k_) for k_ in range(8)] + [R("dec")], [R("Rbf") + "_0"], eng="pool")
            cp("pool", Sst[l][:, h, :], SA[:, 8, :], [SL(8)], [sn])
            for j in range(NT):
                mm(ps[pa][:, j * 128:(j + 1) * 128], vtok[:, j, :], attnm[:, j * 128:(j + 1) * 128], True, False,
                   [R("vtok"), R("attnm")], [PS[pa]])
                for c in range(CPT * j, CPT * (j + 1)):
                    mm(ps[pa][:, c * CH:(c + 1) * CH], Rbf[:, c, :], QT[:, c * CH:(c + 1) * CH], False,
                       c % CPT == CPT - 1, [R("Rbf") + "_%d" % (c // 8), R("QT")], [PS[pa]])
            act(sqH[:], ps[pa][:], AF.Square, [PS[pa]], [R("attnm")])
            mm(ps[pv][:], onesV, sqH[:], True, True, [R("attnm"), "cbb"], [PS[pv]])
            act(L1[:], ps[pv][:], AF.Ln, [PS[pv]], [R("L1")], bias=EPS)
            act(e_t[:], L1[:], AF.Exp, [R("L1")], [R("e_t")], scale=-0.5)
            tt(L2[:], ps[pa][:], e_t[:], ALU.mult, [PS[pa], R("e_t")], [R("L2")])
            stt(mixT[:, 8 + h, :], L2[:], hnw[:, l:l + 1], sgw[:, h, :], ALU.mult, ALU.mult,
                [R("L2"), "hnw", "sgw%d" % h], ["mixT"])
        seq = ([('P', g) for g in range(4)] + [('G', 0), ('G', 1)] + [('H', h) for h in range(8)]) if not INTERLEAVE \
            else [('G', 0), ('H', 0), ('P', 0), ('H', 1), ('P', 1), ('H', 2), ('G', 1), ('H', 3), ('P', 2),
                  ('H', 4), ('P', 3), ('H', 5), ('H', 6), ('H', 7)]
        for kind, idx in seq:
            {'P': unit_P, 'G': unit_G, 'H': unit_H}[kind](idx)
        for hf in range(T // 256):
            marks.append(('O%d l%d' % (hf, l), len(P.ops)))
            u, pb = next_set()
            pms = 4 * (1 - u) + 3
            tsl = slice(hf * 256, (hf + 1) * 256)
            for oc in range(8):
                o = ps[pb + oc // 2][:, (oc % 2) * 256:(oc % 2 + 1) * 256]
                for kc in range(16):
                    mm(o, woutb[:, kc, oc * 128:(oc + 1) * 128], mixT[:, kc, tsl], kc == 0, kc == 15,
                       ["woutb", "mixT"], [PS[pb + oc // 2]])
            for bk in range(4):
                act(hT[:, 2 * bk:2 * bk + 2, tsl], ps[pb + bk][:].rearrange("p (o t) -> p o t", t=256), AF.Square,
                    [PS[pb + bk]], ["hT"])
            for kc in range(8):
                mm(ps[pms][:, 0:256], onesD, hT[:, kc, tsl], kc == 0, kc == 7, ["hT", "cbb"], [PS[pms]])
            act(lnv[:, 0:256], ps[pms][:, 0:256], AF.Ln, [PS[pms]], ["bcum0"], bias=EPS)
            act(rstd[:, 0:256], lnv[:, 0:256], AF.Exp, ["bcum0"], ["bcum1"], scale=-0.5)
            for oc in range(8):
                tb = tO[oc % 2]
                tn = "L1%d" % (oc % 2)
                stt(tb[:, 0:256], ps[pb + oc // 2][:, (oc % 2) * 256:(oc % 2 + 1) * 256], GWcoef[:, l, b, oc:oc + 1],
                    rstd[:, 0:256], ALU.mult, ALU.mult, [PS[pb + oc // 2], "GWcoef%d" % l, "bcum1"], [tn])
                tt(xt[:, oc, tsl], xt[:, oc, tsl], tb[:, 0:256], ALU.add, [xn, tn], [xn],
                   eng=("pool" if oc % 2 == 0 else "dve"))

    out_keys = []
    it = 0
    for b in range(n_seq):
        for l in range(depth):
            P.add("dve", (lambda l=l: lambda e: e.memset(Sst[l][:], 0.0))(), [],
                  ["S%d_%d" % (l, h) for h in range(8)])
        for G in range(n_groups):
            xt = xg[it % 2]
            xn = "xg%d" % (it % 2)
            dma("sp", "x%d" % (it % 2), xt[:], xT_d[b, :, :, G * T:(G + 1) * T], [], [xn], nbytes=2 << 20)
            for l in range(depth):
                layer_pass(l, b, G, xt, xn)
                if it == 0 and l == 0 and depth > 1:
                    ada_layer(1)
            dma("sp", "o%d" % (it % 2), outT_d[b, :, :, G * T:(G + 1) * T], xt[:], [xn], ["out%d" % (it % 2)],
                nbytes=2 << 20)
            it += 1
    P.add("sp", None, ["out0", "out1"], [])
    P.emit(nc, stack)
    stack.close()
    return nc


def _prep_shared(norm_pre_w, ada_w, ada_b, w_in, pool_w, pool_scale, hgrn_lower_bounds, hgrn_norm_w, w_out,
                 norm_post_w):
    f = np.float32
    ada_w = np.asarray(ada_w, f)
    w_in = np.asarray(w_in, f)

    def rows(w):
        L, R, C = w.shape
        return w.reshape(L, R // 128, 128, C).transpose(0, 2, 1, 3)

    adaw = rows(ada_w).reshape(DEPTH, 128, 8, 6, 512).transpose(0, 3, 1, 2, 4)
    win = rows(w_in)
    blocks = []
    for g in range(4):
        blocks.append(np.concatenate([win[..., 256 * g:256 * (g + 1)], win[..., 1024 + 256 * g:1024 + 256 * (g + 1)]],
                                     axis=-1))
    for hb in range(2):
        blocks.append(win[..., 5120 + 512 * hb:5120 + 512 * (hb + 1)])
    wpg = np.stack(blocks, axis=1)
    hq = []
    for h in range(8):
        hq.append(np.concatenate([win[..., 2048 + 128 * h:2048 + 128 * (h + 1)],
                                  win[..., 3072 + 128 * h:3072 + 128 * (h + 1)],
                                  win[..., 4096 + 128 * h:4096 + 128 * (h + 1)]], axis=-1))
    whq = np.stack(hq, axis=1)
    poolw = np.asarray(pool_w, f).reshape(DEPTH, 4, 2, 128, 256).transpose(3, 0, 1, 2, 4)
    wout = np.asarray(w_out, f).reshape(DEPTH, 16, 128, 1024).transpose(0, 2, 1, 3)

    def vec8(v):
        return np.asarray(v, f).reshape(DEPTH, 8, 128).transpose(2, 0, 1)

    mask_scan, mask_bd, cb, band = _consts()
    d = dict(
        adaw=adaw, adab=np.asarray(ada_b, f).reshape(DEPTH, 24, 128).transpose(2, 0, 1),
        wpg=wpg, whq=whq, poolw=poolw, wout=wout,
        npre=vec8(norm_pre_w), pscale=vec8(pool_scale), npost=vec8(norm_post_w),
        hnw=np.asarray(hgrn_norm_w, f).T, lbraw=vec8(hgrn_lower_bounds),
        mscan=mask_scan, mbd=mask_bd, cb=cb, band=band,
    )
    return {k: np.ascontiguousarray(v, dtype=f) for k, v in d.items()}


def kernel(x, c, norm_pre_w, ada_w, ada_b, w_in, pool_w, pool_scale, hgrn_lower_bounds, hgrn_norm_w, w_out,
           norm_post_w):
    x = np.asarray(x, np.float32)
    c = np.asarray(c, np.float32)
    shared = _prep_shared(norm_pre_w, ada_w, ada_b, w_in, pool_w, pool_scale, hgrn_lower_bounds, hgrn_norm_w,
                          w_out, norm_post_w)
    in_maps = []
    for i in range(NCORES):
        xs = x[i * SEQ_PER_CORE:(i + 1) * SEQ_PER_CORE]
        xT = np.ascontiguousarray(xs.reshape(SEQ_PER_CORE, S, 8, 128).transpose(0, 3, 2, 1))
        cs = c[i * SEQ_PER_CORE:(i + 1) * SEQ_PER_CORE]
        cT = np.ascontiguousarray(cs.reshape(SEQ_PER_CORE, 8, 128).transpose(2, 1, 0))
        m = dict(shared)
        m["xT"] = xT
        m["cT"] = cT
        in_maps.append(m)
    nc = build()
    res = run_bass_kernel_spmd(nc, in_maps, core_ids=list(range(NCORES)))
    out = np.empty((B, S, D), np.float32)
    for i in range(NCORES):
        oT = np.asarray(res.results[i]["outT"]).reshape(SEQ_PER_CORE, 128, 8, S)
        out[i * SEQ_PER_CORE:(i + 1) * SEQ_PER_CORE] = oT.transpose(0, 3, 2, 1).reshape(SEQ_PER_CORE, S, D)
    return out
```

```python
import contextlib
import numpy as np
import concourse.bass as bass
import concourse.mybir as mybir
from concourse.bass_utils import run_bass_kernel_spmd

F32 = mybir.dt.float32
BF16 = mybir.dt.bfloat16
AF = mybir.ActivationFunctionType
ALU = mybir.AluOpType

NCORES = 8
B, S, D = 16, 2048, 1024
SEQ_PER_CORE = B // NCORES
DEPTH = 2
T = 512
NG = S // T
NT = T // 128
CH = 32
NCH = T // CH
CPT = 128 // CH
ACLAMP = 5.0e34
EPS = 1e-6
WINDOWS = (2, 4, 8, 16)
NWB = 2
LAT = 300.0
INTERLEAVE = False
USE_RANK = True
ACT_RBF_ALL = True
ENG_LF = "dve"
ENG_T1D = "dve"
KEEP_WARM = False


class Prog:
    def __init__(self):
        self.ops = []
        self.warm = None

    def add(self, eng, fn, reads=(), writes=(), dma=None, cost=300.0, lat=0.0):
        self.ops.append(dict(eng=eng, fn=fn, reads=tuple(reads), writes=tuple(writes), dma=dma,
                             cost=float(cost), lat=float(lat)))

    def _deps(self):
        ops = self.ops
        last_w, readers, last_dma = {}, {}, {}
        for i, op in enumerate(ops):
            op["stream"] = ("dma:" + op["dma"]) if op["dma"] else op["eng"]
            deps = set()
            for r in op["reads"]:
                j = last_w.get(r)
                if j is not None:
                    deps.add(j)
            for w in op["writes"]:
                j = last_w.get(w)
                if j is not None:
                    deps.add(j)
                deps.update(readers.get(w, ()))
            if op["dma"]:
                j = last_dma.get(op["dma"])
                if j is not None:
                    deps.add(j)
                last_dma[op["dma"]] = i
            deps.discard(i)
            for r in op["reads"]:
                readers.setdefault(r, []).append(i)
            for w in op["writes"]:
                last_w[w] = i
                readers[w] = []
            op["deps"] = sorted(deps)

    def _schedule(self, window=24):
        import bisect
        ops = self.ops
        n = len(ops)
        succ = [[] for _ in range(n)]
        rem = [0] * n
        dready = [0.0] * n
        for i, op in enumerate(ops):
            rem[i] = len(op["deps"])
            for j in op["deps"]:
                succ[j].append(i)
        rank = [0.0] * n
        for i in range(n - 1, -1, -1):
            m_ = 0.0
            for s_ in succ[i]:
                if rank[s_] > m_:
                    m_ = rank[s_]
            rank[i] = ops[i]["cost"] + ops[i]["lat"] + m_
        engs = ["pe", "act", "dve", "pool", "sp"]
        ready = {e: [] for e in engs}
        for i, op in enumerate(ops):
            if rem[i] == 0:
                ready[op["eng"]].append(i)
        free = {e: 0.0 for e in engs}
        dma_free = [0.0]
        order = []
        done = 0
        while done < n:
            best = None
            for e in engs:
                rl = ready[e]
                for i in rl[:window]:
                    st = max(free[e], dready[i])
                    key = (st, -rank[i] if (USE_RANK and e != "pe") else i, i)
                    if best is None or key < best[0]:
                        best = (key, i, e)
            i, e = best[1], best[2]
            st = best[0][0]
            ready[e].remove(i)
            op = ops[i]
            op["gap"] = st - free[e]
            free[e] = st + op["cost"]
            op["t0"] = st
            if op["dma"]:
                t0_ = max(st + op["cost"], dma_free[0])
                dma_free[0] = t0_ + op["lat"]
                fin = dma_free[0] + 2000.0
            else:
                fin = st + op["cost"] + op["lat"]
            order.append(i)
            done += 1
            for s_ in succ[i]:
                if fin > dready[s_]:
                    dready[s_] = fin
                rem[s_] -= 1
                if rem[s_] == 0:
                    bisect.insort(ready[ops[s_]["eng"]], s_)
        self.makespan = max(free.values())
        return order

    def finalize(self):
        self._deps()
        order = self._schedule()
        ops = self.ops
        pos = [0] * len(ops)
        for k_, i in enumerate(order):
            pos[i] = k_
        for i, op in enumerate(ops):
            op["signal"] = bool(op["dma"])
        for i, op in enumerate(ops):
            latest = {}
            for j in op["deps"]:
                dj = ops[j]
                if dj["stream"] == op["stream"] and op["eng"] == "pe" and not op["dma"]:
                    continue
                st = dj["stream"]
                if st not in latest or pos[j] > pos[latest[st]]:
                    latest[st] = j
            op["sdeps"] = list(latest.values())
            for j in op["sdeps"]:
                ops[j]["signal"] = True
        tick = {}
        for i in order:
            op = ops[i]
            if op["signal"] and op["fn"] is not None:
                st = op["stream"]
                tick[st] = tick.get(st, 0) + (16 if op["dma"] else 1)
                op["tick"] = tick[st]
        waited = {}
        for i in order:
            op = ops[i]
            need = {}
            for j in op["sdeps"]:
                dj = ops[j]
                need[dj["stream"]] = max(need.get(dj["stream"], 0), dj["tick"])
            w = []
            for st, v in need.items():
                key = (op["eng"], st)
                if waited.get(key, 0) < v:
                    waited[key] = v
                    w.append((st, v))
            op["waits"] = w
        self.order = order
        return sorted(tick.keys())

    def emit(self, nc, stack):
        streams = self.finalize()
        sems = {st: stack.enter_context(nc.semaphore("s_" + st.replace(":", "_"))) for st in streams}
        block = stack.enter_context(nc.Block())
        ops = self.ops
        order = self.order

        def run(engname, e):
            for i in order:
                op = ops[i]
                if op["eng"] != engname:
                    continue
                if engname == "pe" and self.warm is not None and op.get("gap", 0) > 1500.0 and op["waits"]:
                    for _ in range(min(int(0.6 * op["gap"] / 70.0), 96)):
                        e.ldweights(self.warm)
                for st, v in op["waits"]:
                    e.wait_ge(sems[st], v)
                if op["fn"] is None:
                    continue
                inst = op["fn"](e)
                if op["signal"]:
                    inst.then_inc(sems[op["stream"]], 16 if op["dma"] else 1)

        @block.sync
        def _(e):
            run("sp", e)

        @block.gpsimd
        def _(e):
            run("pool", e)

        @block.scalar
        def _(e):
            run("act", e)

        @block.vector
        def _(e):
            run("dve", e)

        @block.tensor
        def _(e):
            run("pe", e)


def _band_mats():
    m = np.zeros((128, 12, 128), np.float32)
    for g, w in enumerate(WINDOWS):
        for tp in range(128):
            for t in range(max(0, tp - w + 1), tp + 1):
                m[t, g, tp] += 1.0 / w
            m[tp, g, tp] -= 1.0
            for t in range(128):
                if t - 128 > tp - w:
                    m[t, 4 + g, tp] += 1.0 / w
            cnt = min(tp + 1, w)
            for t in range(max(0, tp - w + 1), tp + 1):
                m[t, 8 + g, tp] += 1.0 / cnt
            m[tp, 8 + g, tp] -= 1.0
    return m


def _consts():
    mask_scan = np.ones((128, T), np.float32)
    mask_scan[:, ::CH] = 0.0
    bd = np.zeros((128, 128), np.float32)
    for s_ in range(128):
        for t_ in range(128):
            if s_ // CH == t_ // CH and s_ <= t_:
                bd[s_, t_] = 1.0
    mask_bd = np.tile(bd, (1, NT))
    cb = np.zeros((128, 3, 128), np.float32)
    cb[:, 0, :] = np.eye(128, dtype=np.float32)
    cb[:, 1, :] = 1.0 / D
    cb[:, 2, :] = 1.0 / 128
    return mask_scan, mask_bd, cb, _band_mats()


def build(n_seq=SEQ_PER_CORE, n_groups=NG, depth=DEPTH, debug=False, stop=None):
    nc = bass.Bass("TRN2", target_bir_lowering=False)
    P = Prog()
    stack = contextlib.ExitStack()

    def dram(name, shape, kind="ExternalInput"):
        return nc.dram_tensor(name, list(shape), F32, kind=kind).ap()

    xT_d = dram("xT", [SEQ_PER_CORE, 128, 8, S])
    cT_d = dram("cT", [128, 8, SEQ_PER_CORE])
    adaw_d = dram("adaw", [DEPTH, 6, 128, 8, 512])
    adab_d = dram("adab", [128, DEPTH, 24])
    wpg_d = dram("wpg", [DEPTH, 6, 128, 8, 512])
    whq_d = dram("whq", [DEPTH, 8, 128, 8, 384])
    poolw_d = dram("poolw", [128, DEPTH, 4, 2, 256])
    wout_d = dram("wout", [DEPTH, 128, 16, 1024])
    npre_d = dram("npre", [128, DEPTH, 8])
    pscale_d = dram("pscale", [128, DEPTH, 8])
    npost_d = dram("npost", [128, DEPTH, 8])
    hnw_d = dram("hnw", [128, DEPTH])
    lbraw_d = dram("lbraw", [128, DEPTH, 8])
    mscan_d = dram("mscan", [128, T])
    mbd_d = dram("mbd", [128, T])
    cb_d = dram("cb", [128, 3, 128])
    band_d = dram("band", [128, 12, 128])
    outT_d = dram("outT", [SEQ_PER_CORE, 128, 8, S], kind="ExternalOutput")
    dbg_d = {}

    def sb(name, shape, dt=F32):
        return stack.enter_context(nc.sbuf_tensor(name, list(shape), dt))

    ps = [stack.enter_context(nc.psum_tensor("ps%d" % i, [128, 512], F32)) for i in range(8)]
    PS = ["ps%d" % i for i in range(8)]

    xg = [sb("xg%d" % i, [128, 8, T]) for i in range(2)]
    hT = sb("hT", [128, 8, T], BF16)
    mixT = sb("mixT", [128, 16, T], BF16)
    sgw = sb("sgw", [128, 8, T], BF16)
    wbuf = [sb("wbuf%d" % i, [128, 8, 512], BF16) for i in range(NWB)]
    woutb = sb("woutb", [128, 16, 1024], BF16)
    poolwb = sb("poolwb", [128, 4, 2, 256], BF16)
    Sst = [sb("Sst%d" % l, [128, 8, 128]) for l in range(DEPTH)]
    uprev = [sb("uprev%d" % l, [128, 4, 256], BF16) for l in range(DEPTH)]
    mscan = sb("mscan_s", [128, T])
    mbd = sb("mbd_s", [128, T])
    cbb = sb("cbb", [128, 3, 128], BF16)
    bandb = sb("bandb", [128, 12, 128], BF16)
    cT = sb("cT_s", [128, 8, SEQ_PER_CORE])
    scT = sb("scT", [128, 8, SEQ_PER_CORE], BF16)
    adab = sb("adab_s", [128, DEPTH, 24])
    modT = sb("modT", [128, DEPTH, 24, SEQ_PER_CORE])
    npre = sb("npre_s", [128, DEPTH, 8])
    pscale = sb("pscale_s", [128, DEPTH, 8])
    npost = sb("npost_s", [128, DEPTH, 8])
    hnw = sb("hnw_s", [128, DEPTH])
    lbraw = sb("lbraw_s", [128, DEPTH, 8])
    lbe = sb("lbe", [128, DEPTH, 8])
    lbs = sb("lbs", [128, 8])
    lbr = sb("lbr", [128, 8])
    lbsm = sb("lbsm", [128, DEPTH, 8])
    lb = sb("lb", [128, DEPTH, 8])
    ln1mlb = sb("ln1mlb", [128, DEPTH, 8])
    Acoef = sb("Acoef", [128, DEPTH, SEQ_PER_CORE, 8])
    SHcoef = sb("SHcoef", [128, DEPTH, SEQ_PER_CORE, 8])
    GWcoef = sb("GWcoef", [128, DEPTH, SEQ_PER_CORE, 8])
    utok = [sb("utok%d" % i, [128, NT, 256], BF16) for i in range(2)]
    dT = [sb("dT%d" % i, [128, 2, T], BF16) for i in range(2)]
    sgp = [sb("sgp%d" % i, [128, 2, T], BF16) for i in range(2)]
    HS = []
    for i in range(2):
        HS.append(dict(
            e_t=sb("e_t%d" % i, [128, T]),
            L1=sb("L1%d" % i, [128, T]),
            L2=sb("L2%d" % i, [128, T]),
            bcum=sb("bcum%d" % i, [128, T]),
            Dm=sb("Dm%d" % i, [128, T]),
            dec=sb("dec%d" % i, [128, NCH]),
            QT=sb("QT%d" % i, [128, T], BF16),
            KT=sb("KT%d" % i, [128, T], BF16),
            vtok=sb("vtok%d" % i, [128, NT, 128], BF16),
            vblk=sb("vblk%d" % i, [128, NT, CPT, 128], BF16),
            ktok=sb("ktok%d" % i, [128, NT, 128], BF16),
            attnm=sb("attnm%d" % i, [128, T], BF16),
            Rbf=sb("Rbf%d" % i, [128, NCH, 128], BF16),
            Sall=sb("Sall%d" % i, [128, 9, 128]),
        ))
    lnv, rstd = HS[0]["bcum"], HS[1]["bcum"]
    tA = [HS[0]["Dm"], HS[1]["Dm"]]
    tO = [HS[0]["L1"], HS[1]["L1"]]

    ident = cbb[:, 0, :]
    if KEEP_WARM:
        P.warm = cbb[:, 0, :]
    onesD = cbb[:, 1, :]
    onesV = cbb[:, 2, :]

    def fsz(ap):
        n = 1
        for d_ in ap.shape[1:]:
            n *= int(d_)
        return n

    def dma(eng, key, out, in_, reads, writes, nbytes=0):
        P.add(eng, lambda e: e.dma_start(out=out, in_=in_), reads, writes, dma=key,
              cost=(1500.0 if eng == "pool" else 300.0), lat=nbytes / 190.0)

    def mm(out, lhsT, rhs, start, stop, reads, writes):
        P.add("pe", lambda e: e.matmul(out, lhsT=lhsT, rhs=rhs, start=start, stop=stop), reads, writes,
              cost=max(fsz(rhs), 64, min(fsz(lhsT), 128) * 0.6) / 1.7 + 8.0, lat=LAT)

    def tr(out, in_, reads, writes):
        P.add("pe", lambda e: e.transpose(out, in_, ident), reads, writes, cost=80.0, lat=LAT)

    def act(out, in_, func, reads, writes, scale=None, bias=None):
        kw = {}
        if scale is not None:
            kw["scale"] = scale
        if bias is not None:
            kw["bias"] = bias
        P.add("act", lambda e: e.activation(out=out, in_=in_, func=func, **kw), reads, writes,
              cost=220.0 + fsz(in_) / 1.3, lat=LAT)

    def tt(out, in0, in1, op, reads, writes, eng="dve"):
        c = 80.0 + fsz(in0) * 1.6 if eng == "dve" else 200.0 + fsz(in0) * 3.0
        P.add(eng, lambda e: e.tensor_tensor(out=out, in0=in0, in1=in1, op=op), reads, writes, cost=c, lat=LAT)

    def ts(out, in0, s1, op0, reads, writes, s2=None, op1=None, eng="dve"):
        c = 80.0 + fsz(in0) * 1.1 if eng == "dve" else 200.0 + fsz(in0) * 2.5
        if op1 is None:
            P.add(eng, lambda e: e.tensor_scalar(out=out, in0=in0, scalar1=s1, scalar2=None, op0=op0), reads, writes,
                  cost=c, lat=LAT)
        else:
            P.add(eng, lambda e: e.tensor_scalar(out=out, in0=in0, scalar1=s1, scalar2=s2, op0=op0, op1=op1),
                  reads, writes, cost=c, lat=LAT)

    def stt(out, in0, scalar, in1, op0, op1, reads, writes):
        P.add("dve", lambda e: e.scalar_tensor_tensor(out=out, in0=in0, scalar=scalar, in1=in1, op0=op0, op1=op1),
              reads, writes, cost=120.0 + fsz(in0) * 1.6, lat=LAT)

    def cp(eng, out, in_, reads, writes):
        c = 80.0 + fsz(in_) * 1.1 if eng == "dve" else 200.0 + fsz(in_) * 2.5
        P.add(eng, lambda e: e.tensor_copy(out=out, in_=in_), reads, writes, cost=c, lat=LAT)

    dma("sp", "c0", cT[:], cT_d, [], ["cT"])
    dma("sp", "c1", adab[:], adab_d, [], ["adab"])
    dma("sp", "c2", npre[:], npre_d, [], ["npre"])
    dma("sp", "c3", pscale[:], pscale_d, [], ["pscale"])
    dma("sp", "c4", npost[:], npost_d, [], ["npost"])
    dma("sp", "c5", hnw[:], hnw_d, [], ["hnw"])
    dma("sp", "c6", lbraw[:], lbraw_d, [], ["lbraw"])
    dma("sp", "c7", mscan[:], mscan_d, [], ["mscan"])
    dma("sp", "c8", mbd[:], mbd_d, [], ["mbd"])
    dma("pool", "c9", cbb[:], cb_d, [], ["cbb"])
    dma("pool", "c10", bandb[:], band_d, [], ["bandb"])

    wcount = [0]

    def load_w(src, ncols):
        i = wcount[0] % NWB
        wcount[0] += 1
        dma("pool", "w%d" % i, wbuf[i][:, :, 0:ncols], src, [], ["wbuf%d" % i], nbytes=128 * 8 * ncols * 4)
        return wbuf[i], "wbuf%d" % i

    for i_ in range(2):
        P.add("dve", (lambda i_=i_: lambda e: e.memset(HS[i_]["vblk"][:], 0.0))(), [], ["vblk%d" % i_])
    act(scT[:], cT[:], AF.Silu, ["cT"], ["scT"])
    def ada_layer(l):
        for blk in range(6):
            wb, wn = load_w(adaw_d[l, blk], 512)
            for j in range(4):
                col = blk * 4 + j
                o = ps[0][:, (l * 24 + col) * SEQ_PER_CORE:(l * 24 + col + 1) * SEQ_PER_CORE]
                for kc in range(8):
                    mm(o, wb[:, kc, j * 128:(j + 1) * 128], scT[:, kc, :], kc == 0, kc == 7,
                       [wn, "scT"], [PS[0]])
        tt(modT[:, l], ps[0][:, l * 24 * SEQ_PER_CORE:(l + 1) * 24 * SEQ_PER_CORE].rearrange(
            "p (c b) -> p c b", b=SEQ_PER_CORE),
           adab[:, l, :].unsqueeze(2).to_broadcast([128, 24, SEQ_PER_CORE]), ALU.add,
           [PS[0], "adab"], ["modT%d" % l])
        for b in range(SEQ_PER_CORE):
            stt(Acoef[:, l, b, :], modT[:, l, 8:16, b], 1.0, npre[:, l, :], ALU.add, ALU.mult,
                ["modT%d" % l, "npre"], ["Acoef%d" % l])
            P.add("dve", (lambda l=l, b=b: lambda e: e.tensor_copy(out=SHcoef[:, l, b, :], in_=modT[:, l, 0:8, b]))(),
                  ["modT%d" % l], ["SHcoef%d" % l])
            tt(GWcoef[:, l, b, :], modT[:, l, 16:24, b], npost[:, l, :], ALU.mult, ["modT%d" % l, "npost"],
               ["GWcoef%d" % l])

    ada_layer(0)
    act(lbe[:], lbraw[:], AF.Exp, ["lbraw"], ["lbe"])
    tt(lbs[:], lbe[:, 0, :], lbe[:, 1, :], ALU.add, ["lbe"], ["lbs"])
    P.add("dve", lambda e: e.reciprocal(out=lbr[:], in_=lbs[:]), ["lbs"], ["lbr"])
    for l in range(DEPTH):
        tt(lbsm[:, l, :], lbe[:, l, :], lbr[:], ALU.mult, ["lbe", "lbr"], ["lbsm"])
    tt(lb[:, 0, :], lbsm[:, 0, :], lbsm[:, 0, :], ALU.subtract, ["lbsm"], ["lb0"])
    tt(lbs[:], lbsm[:, 0, :], lbsm[:, 1, :], ALU.add, ["lbsm", "lbr"], ["lbs2"])
    tt(lb[:, 1, :], lbs[:], lbsm[:, 0, :], ALU.subtract, ["lbs2", "lbsm"], ["lb1"])
    act(ln1mlb[:], lb[:], AF.Ln, ["lb0", "lb1"], ["ln1mlb"], scale=-1.0, bias=1.0)
    LBR = ["lb0", "lb1", "ln1mlb"]

    unit = [0]
    marks = []
    P.marks = marks

    def next_set():
        u = unit[0] % 2
        unit[0] += 1
        return u, 4 * u

    def layer_pass(l, b, G, xt, xn):
        first = (G == 0)
        marks.append(('A l%d b%d G%d' % (l, b, G), len(P.ops)))
        dma("pool", "wo", woutb[:], wout_d[l], [], ["woutb"], nbytes=8 << 20)
        dma("pool", "pw", poolwb[:], poolw_d[:, l], [], ["poolwb"], nbytes=512 << 10)
        u, pb = next_set()
        sqA = mixT[:, 0:8, :]
        for k2 in range(4):
            act(mixT[:, 2 * k2:2 * k2 + 2, :], xt[:, 2 * k2:2 * k2 + 2, :], AF.Square, [xn], ["mixT"])
            for kc in (2 * k2, 2 * k2 + 1):
                mm(ps[pb + 3][:], onesD, mixT[:, kc, :], kc == 0, kc == 7, ["mixT", "cbb"], [PS[pb + 3]])
        act(lnv[:], ps[pb + 3][:], AF.Ln, [PS[pb + 3]], ["bcum0"], bias=EPS)
        act(rstd[:], lnv[:], AF.Exp, ["bcum0"], ["bcum1"], scale=-0.5)
        for kc in range(8):
            tb = tA[kc % 2]
            tn = "Dm%d" % (kc % 2)
            tt(tb[:], xt[:, kc, :], rstd[:], ALU.mult, [xn, "bcum1"], [tn], eng=("dve" if kc % 4 != 3 else "pool"))
            act(hT[:, kc, :], tb[:], AF.Identity, [tn, "Acoef%d" % l, "SHcoef%d" % l], ["hT"],
                scale=Acoef[:, l, b, kc:kc + 1], bias=SHcoef[:, l, b, kc:kc + 1])
        if stop == 'A':
            return
        def unit_P(g):
            marks.append(('P%d l%d' % (g, l), len(P.ops)))
            u, pb = next_set()
            wb, wn = load_w(wpg_d[l, g], 512)
            un, dn, sn_ = "utok%d" % u, "dT%d" % u, "sgp%d" % u
            for oc in range(2):
                for kc in range(8):
                    mm(ps[pb + oc][:], wb[:, kc, 256 + oc * 128:256 + (oc + 1) * 128], hT[:, kc, :], kc == 0, kc == 7,
                       [wn, "hT"], [PS[pb + oc]])
            for j in range(NT):
                o = ps[pb + 2 + j // 2][:, (j % 2) * 256:(j % 2 + 1) * 256]
                for kc in range(8):
                    mm(o, hT[:, kc, j * 128:(j + 1) * 128], wb[:, kc, 0:256], kc == 0, kc == 7,
                       [wn, "hT"], [PS[pb + 2 + j // 2]])
            for oc in range(2):
                act(sgp[u][:, oc, :], ps[pb + oc][:], AF.Silu, [PS[pb + oc]], [sn_])
            for jj in range(NT // 2):
                cp("dve", utok[u][:, 2 * jj:2 * jj + 2, :], ps[pb + 2 + jj][:].rearrange("p (j c) -> p j c", c=256),
                   [PS[pb + 2 + jj]], [un])
            for cc in range(2):
                bk = pb + 2 + cc
                for j in range(NT):
                    o = ps[bk][:, j * 128:(j + 1) * 128]
                    if j == 0:
                        if first:
                            mm(o, utok[u][:, 0, cc * 128:(cc + 1) * 128], bandb[:, 8 + g, :], True, True,
                               [un, "bandb"], [PS[bk]])
                        else:
                            mm(o, utok[u][:, 0, cc * 128:(cc + 1) * 128], bandb[:, g, :], True, False,
                               [un, "bandb"], [PS[bk]])
                            mm(o, uprev[l][:, g, cc * 128:(cc + 1) * 128], bandb[:, 4 + g, :], False, True,
                               ["uprev%d_%d" % (l, g), "bandb"], [PS[bk]])
                    else:
                        mm(o, utok[u][:, j, cc * 128:(cc + 1) * 128], bandb[:, g, :], True, False,
                           [un, "bandb"], [PS[bk]])
                        mm(o, utok[u][:, j - 1, cc * 128:(cc + 1) * 128], bandb[:, 4 + g, :], False, True,
                           [un, "bandb"], [PS[bk]])
            cp("pool", uprev[l][:, g, :], utok[u][:, NT - 1, :], [un], ["uprev%d_%d" % (l, g)])
            for cc in range(2):
                act(dT[u][:, cc, :], ps[pb + 2 + cc][:], AF.Copy, [PS[pb + 2 + cc]], [dn])
            for oc in range(2):
                for cc in range(2):
                    mm(ps[pb + oc][:], poolwb[:, g, cc, oc * 128:(oc + 1) * 128], dT[u][:, cc, :], cc == 0, cc == 1,
                       ["poolwb", dn], [PS[pb + oc]])
            for oc in range(2):
                stt(mixT[:, 2 * g + oc, :], ps[pb + oc][:], pscale[:, l, 2 * g + oc:2 * g + oc + 1], sgp[u][:, oc, :],
                    ALU.mult, ALU.mult, [PS[pb + oc], "pscale", sn_], ["mixT"])
        def unit_G(hb):
            marks.append(('G%d l%d' % (hb, l), len(P.ops)))
            u, pb = next_set()
            wb, wn = load_w(wpg_d[l, 4 + hb], 512)
            for oc in range(4):
                for kc in range(8):
                    mm(ps[pb + oc][:], wb[:, kc, oc * 128:(oc + 1) * 128], hT[:, kc, :], kc == 0, kc == 7,
                       [wn, "hT"], [PS[pb + oc]])
            for oc in range(4):
                act(sgw[:, hb * 4 + oc, :], ps[pb + oc][:], AF.Silu, [PS[pb + oc]], ["sgw%d" % (hb * 4 + oc)])
        def unit_H(h):
            marks.append(('H%d l%d' % (h, l), len(P.ops)))
            u, pb = next_set()
            pq, pf, pv, pa = pb, pb + 1, pb + 2, pb + 3
            X = HS[u]
            e_t, L1, L2, bcum, Dm, dec = X["e_t"], X["L1"], X["L2"], X["bcum"], X["Dm"], X["dec"]
            QT, KT, vtok, vblk, ktok, attnm, Rbf, SA, sqH = (X["QT"], X["KT"], X["vtok"], X["vblk"], X["ktok"],
                                                           X["attnm"], X["Rbf"], X["Sall"], X["attnm"])

            def R(nm, u=u):
                return "%s%d" % (nm, u)

            wb, wn = load_w(whq_d[l, h], 384)
            for kc in range(8):
                mm(ps[pq][:], wb[:, kc, 0:128], hT[:, kc, :], kc == 0, kc == 7, [wn, "hT"], [PS[pq]])
            for kc in range(8):
                mm(ps[pf][:], wb[:, kc, 128:256], hT[:, kc, :], kc == 0, kc == 7, [wn, "hT"], [PS[pf]])
            for j in range(NT):
                for kc in range(8):
                    mm(ps[pv][:, j * 128:(j + 1) * 128], hT[:, kc, j * 128:(j + 1) * 128], wb[:, kc, 256:384],
                       kc == 0, kc == 7, [wn, "hT"], [PS[pv]])
            act(e_t[:], ps[pf][:], AF.Exp, [PS[pf]], [R("e_t")], scale=-1.0)
            act(L2[:], e_t[:], AF.Ln, [R("e_t")], [R("L2")], bias=1.0)
            act(L1[:], e_t[:], AF.Ln, [R("e_t")] + LBR, [R("L1")], scale=lb[:, l, h:h + 1], bias=1.0)
            act(vtok[:], ps[pv][:].rearrange("p (j c) -> p j c", c=128), AF.Copy, [PS[pv]], [R("vtok")])
            for q in range(CPT):
                act(vblk[q * CH:(q + 1) * CH, :, q, :], ps[pv][q * CH:(q + 1) * CH, :].rearrange("p (j c) -> p j c", c=128),
                    AF.Copy, [PS[pv]], [R("vblk")])
            tt(L1[:], L1[:], L2[:], ALU.subtract, [R("L1"), R("L2")], [R("L1")], eng=ENG_LF)
            P.add("dve", (lambda bcum=bcum, L1=L1: lambda e: e.tensor_tensor_scan(
                out=bcum[:], data0=mscan[:], data1=L1[:], initial=0.0, op0=ALU.mult, op1=ALU.add))(),
                [R("L1"), "mscan"], [R("bcum")], cost=80.0 + T * 2.1, lat=LAT)
            b3 = bcum[:].rearrange("p (c t) -> p c t", t=CH)
            tt(Dm[:].rearrange("p (c t) -> p c t", t=CH), b3, b3[:, :, CH - 1:CH].to_broadcast([128, NCH, CH]),
               ALU.subtract, [R("bcum")], [R("Dm")])
            act(dec[:], b3[:, :, CH - 1], AF.Exp, [R("bcum")], [R("dec")])
            act(e_t[:], Dm[:], AF.Exp, [R("Dm")], [R("e_t")])
            stt(QT[:], e_t[:], ACLAMP, ps[pq][:], ALU.min, ALU.mult, [PS[pq], R("e_t")], [R("QT")])
            tt(L2[:], ps[pf][:], L2[:], ALU.add, [PS[pf], R("L2")], [R("L2")])
            tt(L2[:], L2[:], Dm[:], ALU.add, [R("L2"), R("Dm")], [R("L2")], eng=ENG_T1D)
            act(KT[:], L2[:], AF.Exp, [R("L2")] + LBR, [R("KT")], scale=-1.0, bias=ln1mlb[:, l, h:h + 1])
            ktp = ps[pv][:].bitcast(BF16)
            for j in range(NT):
                tr(ktp[:, j * 128:(j + 1) * 128], KT[:, j * 128:(j + 1) * 128], [R("KT"), "cbb"], [PS[pv]])
            act(ktok[:].rearrange("p j c -> p (j c)"), ktp[:, 0:T], AF.Copy, [PS[pv]], [R("ktok")])
            for j in range(NT):
                mm(ps[pa][:, j * 128:(j + 1) * 128], KT[:, j * 128:(j + 1) * 128], QT[:, j * 128:(j + 1) * 128],
                   True, True, [R("KT"), R("QT")], [PS[pa]])
            tt(attnm[:], ps[pa][:], mbd[:], ALU.mult, [PS[pa], "mbd"], [R("attnm")])
            sn = "S%d_%d" % (l, h)

            def SL(k_):
                return "%s_%d" % (R("Sall"), k_)

            for hh in range(2):
                if hh == 0:
                    cp("pool", SA[:, 0, :], Sst[l][:, h, :], [sn], [SL(0)])
                else:
                    cp("pool", SA[:, 0, :], SA[:, 8, :], [SL(8)], [SL(0)])
                for jj in range(2):
                    j = 2 * hh + jj
                    bank = (pq, pf)[jj]
                    mm(ps[bank][:], ktok[:, j, :], vblk[:, j, :, :].rearrange("p q c -> p (q c)"), True, True,
                       [R("ktok"), R("vblk")], [PS[bank]])
                for i_ in range(8):
                    c = 8 * hh + i_
                    bank = (pq, pf)[i_ // 4]
                    if hh == 1 or ACT_RBF_ALL:
                        act(Rbf[:, c, :], SA[:, i_, :], AF.Identity, [SL(i_), R("dec")], [R("Rbf") + "_%d" % hh],
                            scale=dec[:, c:c + 1])
                    stt(SA[:, i_ + 1, :], SA[:, i_, :], dec[:, c:c + 1], ps[bank][:, (i_ % 4) * 128:(i_ % 4 + 1) * 128],
                        ALU.mult, ALU.add, [SL(i_), R("dec"), PS[bank]], [SL(i_ + 1)])
                if hh == 0 and not ACT_RBF_ALL:
                    tt(Rbf[:, 0:8, :], SA[:, 0:8, :],
                       dec[:, 0:8].unsqueeze(2).to_broadcast([128, 8, 128]), ALU.mult,
                       [SL(k_) for k_ in range(8)] + [R("dec")], [R("Rbf") + "_0"], eng="pool")
            cp("pool", Sst[l][:, h, :], SA[:, 8, :], [SL(8)], [sn])
            for j in range(NT):
                mm(ps[pa][:, j * 128:(j + 1) * 128], vtok[:, j, :], attnm[:, j * 128:(j + 1) * 128], True, False,
                   [R("vtok"), R("attnm")], [PS[pa]])
                for c in range(CPT * j, CPT * (j + 1)):
                    mm(ps[pa][:, c * CH:(c + 1) * CH], Rbf[:, c, :], QT[:, c * CH:(c + 1) * CH], False,
                       c % CPT == CPT - 1, [R("Rbf") + "_%d" % (c // 8), R("QT")], [PS[pa]])
            act(sqH[:], ps[pa][:], AF.Square, [PS[pa]], [R("attnm")])
            mm(ps[pv][:], onesV, sqH[:], True, True, [R("attnm"), "cbb"], [PS[pv]])
            act(L1[:], ps[pv][:], AF.Ln, [PS[pv]], [R("L1")], bias=EPS)
            act(e_t[:], L1[:], AF.Exp, [R("L1")], [R("e_t")], scale=-0.5)
            tt(L2[:], ps[pa][:], e_t[:], ALU.mult, [PS[pa], R("e_t")], [R("L2")])
            stt(mixT[:, 8 + h, :], L2[:], hnw[:, l:l + 1], sgw[:, h, :], ALU.mult, ALU.mult,
                [R("L2"), "hnw", "sgw%d" % h], ["mixT"])
        seq = ([('P', g) for g in range(4)] + [('G', 0), ('G', 1)] + [('H', h) for h in range(8)]) if not INTERLEAVE \
            else [('G', 0), ('H', 0), ('P', 0), ('H', 1), ('P', 1), ('H', 2), ('G', 1), ('H', 3), ('P', 2),
                  ('H', 4), ('P', 3), ('H', 5), ('H', 6), ('H', 7)]
        for kind, idx in seq:
            {'P': unit_P, 'G': unit_G, 'H': unit_H}[kind](idx)
        for hf in range(T // 256):
            marks.append(('O%d l%d' % (hf, l), len(P.ops)))
            u, pb = next_set()
            pms = 4 * (1 - u) + 3
            tsl = slice(hf * 256, (hf + 1) * 256)
            for oc in range(8):
                o = ps[pb + oc // 2][:, (oc % 2) * 256:(oc % 2 + 1) * 256]
                for kc in range(16):
                    mm(o, woutb[:, kc, oc * 128:(oc + 1) * 128], mixT[:, kc, tsl], kc == 0, kc == 15,
                       ["woutb", "mixT"], [PS[pb + oc // 2]])
            for bk in range(4):
                act(hT[:, 2 * bk:2 * bk + 2, tsl], ps[pb + bk][:].rearrange("p (o t) -> p o t", t=256), AF.Square,
                    [PS[pb + bk]], ["hT"])
            for kc in range(8):
                mm(ps[pms][:, 0:256], onesD, hT[:, kc, tsl], kc == 0, kc == 7, ["hT", "cbb"], [PS[pms]])
            act(lnv[:, 0:256], ps[pms][:, 0:256], AF.Ln, [PS[pms]], ["bcum0"], bias=EPS)
            act(rstd[:, 0:256], lnv[:, 0:256], AF.Exp, ["bcum0"], ["bcum1"], scale=-0.5)
            for oc in range(8):
                tb = tO[oc % 2]
                tn = "L1%d" % (oc % 2)
                stt(tb[:, 0:256], ps[pb + oc // 2][:, (oc % 2) * 256:(oc % 2 + 1) * 256], GWcoef[:, l, b, oc:oc + 1],
                    rstd[:, 0:256], ALU.mult, ALU.mult, [PS[pb + oc // 2], "GWcoef%d" % l, "bcum1"], [tn])
                tt(xt[:, oc, tsl], xt[:, oc, tsl], tb[:, 0:256], ALU.add, [xn, tn], [xn],
                   eng=("pool" if oc % 2 == 0 else "dve"))

    out_keys = []
    it = 0
    for b in range(n_seq):
        for l in range(depth):
            P.add("dve", (lambda l=l: lambda e: e.memset(Sst[l][:], 0.0))(), [],
                  ["S%d_%d" % (l, h) for h in range(8)])
        for G in range(n_groups):
            xt = xg[it % 2]
            xn = "xg%d" % (it % 2)
            dma("sp", "x%d" % (it % 2), xt[:], xT_d[b, :, :, G * T:(G + 1) * T], [], [xn], nbytes=2 << 20)
            for l in range(depth):
                layer_pass(l, b, G, xt, xn)
                if it == 0 and l == 0 and depth > 1:
                    ada_layer(1)
            dma("sp", "o%d" % (it % 2), outT_d[b, :, :, G * T:(G + 1) * T], xt[:], [xn], ["out%d" % (it % 2)],
                nbytes=2 << 20)
            it += 1
    P.add("sp", None, ["out0", "out1"], [])
    P.emit(nc, stack)
    stack.close()
    return nc


def _prep_shared(norm_pre_w, ada_w, ada_b, w_in, pool_w, pool_scale, hgrn_lower_bounds, hgrn_norm_w, w_out,
                 norm_post_w):
    f = np.float32
    ada_w = np.asarray(ada_w, f)
    w_in = np.asarray(w_in, f)

    def rows(w):
        L, R, C = w.shape
        return w.reshape(L, R // 128, 128, C).transpose(0, 2, 1, 3)

    adaw = rows(ada_w).reshape(DEPTH, 128, 8, 6, 512).transpose(0, 3, 1, 2, 4)
    win = rows(w_in)
    blocks = []
    for g in range(4):
        blocks.append(np.concatenate([win[..., 256 * g:256 * (g + 1)], win[..., 1024 + 256 * g:1024 + 256 * (g + 1)]],
                                     axis=-1))
    for hb in range(2):
        blocks.append(win[..., 5120 + 512 * hb:5120 + 512 * (hb + 1)])
    wpg = np.stack(blocks, axis=1)
    hq = []
    for h in range(8):
        hq.append(np.concatenate([win[..., 2048 + 128 * h:2048 + 128 * (h + 1)],
                                  win[..., 3072 + 128 * h:3072 + 128 * (h + 1)],
                                  win[..., 4096 + 128 * h:4096 + 128 * (h + 1)]], axis=-1))
    whq = np.stack(hq, axis=1)
    poolw = np.asarray(pool_w, f).reshape(DEPTH, 4, 2, 128, 256).transpose(3, 0, 1, 2, 4)
    wout = np.asarray(w_out, f).reshape(DEPTH, 16, 128, 1024).transpose(0, 2, 1, 3)

    def vec8(v):
        return np.asarray(v, f).reshape(DEPTH, 8, 128).transpose(2, 0, 1)

    mask_scan, mask_bd, cb, band = _consts()
    d = dict(
        adaw=adaw, adab=np.asarray(ada_b, f).reshape(DEPTH, 24, 128).transpose(2, 0, 1),
        wpg=wpg, whq=whq, poolw=poolw, wout=wout,
        npre=vec8(norm_pre_w), pscale=vec8(pool_scale), npost=vec8(norm_post_w),
        hnw=np.asarray(hgrn_norm_w, f).T, lbraw=vec8(hgrn_lower_bounds),
        mscan=mask_scan, mbd=mask_bd, cb=cb, band=band,
    )
    return {k: np.ascontiguousarray(v, dtype=f) for k, v in d.items()}


def kernel(x, c, norm_pre_w, ada_w, ada_b, w_in, pool_w, pool_scale, hgrn_lower_bounds, hgrn_norm_w, w_out,
           norm_post_w):
    x = np.asarray(x, np.float32)
    c = np.asarray(c, np.float32)
    shared = _prep_shared(norm_pre_w, ada_w, ada_b, w_in, pool_w, pool_scale, hgrn_lower_bounds, hgrn_norm_w,
                          w_out, norm_post_w)
    in_maps = []
    for i in range(NCORES):
        xs = x[i * SEQ_PER_CORE:(i + 1) * SEQ_PER_CORE]
        xT = np.ascontiguousarray(xs.reshape(SEQ_PER_CORE, S, 8, 128).transpose(0, 3, 2, 1))
        cs = c[i * SEQ_PER_CORE:(i + 1) * SEQ_PER_CORE]
        cT = np.ascontiguousarray(cs.reshape(SEQ_PER_CORE, 8, 128).transpose(2, 1, 0))
        m = dict(shared)
        m["xT"] = xT
        m["cT"] = cT
        in_maps.append(m)
    nc = build()
    res = run_bass_kernel_spmd(nc, in_maps, core_ids=list(range(NCORES)))
    out = np.empty((B, S, D), np.float32)
    for i in range(NCORES):
        oT = np.asarray(res.results[i]["outT"]).reshape(SEQ_PER_CORE, 128, 8, S)
        out[i * SEQ_PER_CORE:(i + 1) * SEQ_PER_CORE] = oT.transpose(0, 3, 2, 1).reshape(SEQ_PER_CORE, S, D)
    return out
```

```python
import contextlib
import numpy as np
import concourse.bass as bass
import concourse.mybir as mybir
from concourse.bass_utils import run_bass_kernel_spmd

F32 = mybir.dt.float32
BF16 = mybir.dt.bfloat16
AF = mybir.ActivationFunctionType
ALU = mybir.AluOpType

NCORES = 8
B, S, D = 16, 2048, 1024
SEQ_PER_CORE = B // NCORES
DEPTH = 2
T = 512
NG = S // T
NT = T // 128
CH = 32
NCH = T // CH
CPT = 128 // CH
ACLAMP = 5.0e34
EPS = 1e-6
WINDOWS = (2, 4, 8, 16)
NWB = 2
LAT = 300.0
INTERLEAVE = False
ACT_RBF_ALL = True
ENG_LF = "dve"
ENG_T1D = "dve"
KEEP_WARM = False


class Prog:
    def __init__(self):
        self.ops = []
        self.warm = None

    def add(self, eng, fn, reads=(), writes=(), dma=None, cost=300.0, lat=0.0):
        self.ops.append(dict(eng=eng, fn=fn, reads=tuple(reads), writes=tuple(writes), dma=dma,
                             cost=float(cost), lat=float(lat)))

    def _deps(self):
        ops = self.ops
        last_w, readers, last_dma = {}, {}, {}
        for i, op in enumerate(ops):
            op["stream"] = ("dma:" + op["dma"]) if op["dma"] else op["eng"]
            deps = set()
            for r in op["reads"]:
                j = last_w.get(r)
                if j is not None:
                    deps.add(j)
            for w in op["writes"]:
                j = last_w.get(w)
                if j is not None:
                    deps.add(j)
                deps.update(readers.get(w, ()))
            if op["dma"]:
                j = last_dma.get(op["dma"])
                if j is not None:
                    deps.add(j)
                last_dma[op["dma"]] = i
            deps.discard(i)
            for r in op["reads"]:
                readers.setdefault(r, []).append(i)
            for w in op["writes"]:
                last_w[w] = i
                readers[w] = []
            op["deps"] = sorted(deps)

    def _schedule(self, window=24):
        import bisect
        ops = self.ops
        n = len(ops)
        succ = [[] for _ in range(n)]
        rem = [0] * n
        dready = [0.0] * n
        for i, op in enumerate(ops):
            rem[i] = len(op["deps"])
            for j in op["deps"]:
                succ[j].append(i)
        engs = ["pe", "act", "dve", "pool", "sp"]
        ready = {e: [] for e in engs}
        for i, op in enumerate(ops):
            if rem[i] == 0:
                ready[op["eng"]].append(i)
        free = {e: 0.0 for e in engs}
        dma_free = [0.0]
        order = []
        done = 0
        while done < n:
            best = None
            for e in engs:
                rl = ready[e]
                for i in rl[:window]:
                    st = max(free[e], dready[i])
                    key = (st, i)
                    if best is None or key < best[0]:
                        best = (key, i, e)
            (st, _), i, e = best
            ready[e].remove(i)
            op = ops[i]
            op["gap"] = st - free[e]
            free[e] = st + op["cost"]
            op["t0"] = st
            if op["dma"]:
                t0_ = max(st + op["cost"], dma_free[0])
                dma_free[0] = t0_ + op["lat"]
                fin = dma_free[0] + 2000.0
            else:
                fin = st + op["cost"] + op["lat"]
            order.append(i)
            done += 1
            for s_ in succ[i]:
                if fin > dready[s_]:
                    dready[s_] = fin
                rem[s_] -= 1
                if rem[s_] == 0:
                    bisect.insort(ready[ops[s_]["eng"]], s_)
        self.makespan = max(free.values())
        return order

    def finalize(self):
        self._deps()
        order = self._schedule()
        ops = self.ops
        pos = [0] * len(ops)
        for k_, i in enumerate(order):
            pos[i] = k_
        for i, op in enumerate(ops):
            op["signal"] = bool(op["dma"])
        for i, op in enumerate(ops):
            latest = {}
            for j in op["deps"]:
                dj = ops[j]
                if dj["stream"] == op["stream"] and op["eng"] == "pe" and not op["dma"]:
                    continue
                st = dj["stream"]
                if st not in latest or pos[j] > pos[latest[st]]:
                    latest[st] = j
            op["sdeps"] = list(latest.values())
            for j in op["sdeps"]:
                ops[j]["signal"] = True
        tick = {}
        for i in order:
            op = ops[i]
            if op["signal"] and op["fn"] is not None:
                st = op["stream"]
                tick[st] = tick.get(st, 0) + (16 if op["dma"] else 1)
                op["tick"] = tick[st]
        waited = {}
        for i in order:
            op = ops[i]
            need = {}
            for j in op["sdeps"]:
                dj = ops[j]
                need[dj["stream"]] = max(need.get(dj["stream"], 0), dj["tick"])
            w = []
            for st, v in need.items():
                key = (op["eng"], st)
                if waited.get(key, 0) < v:
                    waited[key] = v
                    w.append((st, v))
            op["waits"] = w
        self.order = order
        return sorted(tick.keys())

    def emit(self, nc, stack):
        streams = self.finalize()
        sems = {st: stack.enter_context(nc.semaphore("s_" + st.replace(":", "_"))) for st in streams}
        block = stack.enter_context(nc.Block())
        ops = self.ops
        order = self.order

        def run(engname, e):
            for i in order:
                op = ops[i]
                if op["eng"] != engname:
                    continue
                if engname == "pe" and self.warm is not None and op.get("gap", 0) > 1500.0 and op["waits"]:
                    for _ in range(min(int(0.6 * op["gap"] / 70.0), 96)):
                        e.ldweights(self.warm)
                for st, v in op["waits"]:
                    e.wait_ge(sems[st], v)
                if op["fn"] is None:
                    continue
                inst = op["fn"](e)
                if op["signal"]:
                    inst.then_inc(sems[op["stream"]], 16 if op["dma"] else 1)

        @block.sync
        def _(e):
            run("sp", e)

        @block.gpsimd
        def _(e):
            run("pool", e)

        @block.scalar
        def _(e):
            run("act", e)

        @block.vector
        def _(e):
            run("dve", e)

        @block.tensor
        def _(e):
            run("pe", e)


def _band_mats():
    m = np.zeros((128, 12, 128), np.float32)
    for g, w in enumerate(WINDOWS):
        for tp in range(128):
            for t in range(max(0, tp - w + 1), tp + 1):
                m[t, g, tp] += 1.0 / w
            m[tp, g, tp] -= 1.0
            for t in range(128):
                if t - 128 > tp - w:
                    m[t, 4 + g, tp] += 1.0 / w
            cnt = min(tp + 1, w)
            for t in range(max(0, tp - w + 1), tp + 1):
                m[t, 8 + g, tp] += 1.0 / cnt
            m[tp, 8 + g, tp] -= 1.0
    return m


def _consts():
    mask_scan = np.ones((128, T), np.float32)
    mask_scan[:, ::CH] = 0.0
    bd = np.zeros((128, 128), np.float32)
    for s_ in range(128):
        for t_ in range(128):
            if s_ // CH == t_ // CH and s_ <= t_:
                bd[s_, t_] = 1.0
    mask_bd = np.tile(bd, (1, NT))
    cb = np.zeros((128, 3, 128), np.float32)
    cb[:, 0, :] = np.eye(128, dtype=np.float32)
    cb[:, 1, :] = 1.0 / D
    cb[:, 2, :] = 1.0 / 128
    return mask_scan, mask_bd, cb, _band_mats()


def build(n_seq=SEQ_PER_CORE, n_groups=NG, depth=DEPTH, debug=False, stop=None):
    nc = bass.Bass("TRN2", target_bir_lowering=False)
    P = Prog()
    stack = contextlib.ExitStack()

    def dram(name, shape, kind="ExternalInput"):
        return nc.dram_tensor(name, list(shape), F32, kind=kind).ap()

    xT_d = dram("xT", [SEQ_PER_CORE, 128, 8, S])
    cT_d = dram("cT", [128, 8, SEQ_PER_CORE])
    adaw_d = dram("adaw", [DEPTH, 6, 128, 8, 512])
    adab_d = dram("adab", [128, DEPTH, 24])
    wpg_d = dram("wpg", [DEPTH, 6, 128, 8, 512])
    whq_d = dram("whq", [DEPTH, 8, 128, 8, 384])
    poolw_d = dram("poolw", [128, DEPTH, 4, 2, 256])
    wout_d = dram("wout", [DEPTH, 128, 16, 1024])
    npre_d = dram("npre", [128, DEPTH, 8])
    pscale_d = dram("pscale", [128, DEPTH, 8])
    npost_d = dram("npost", [128, DEPTH, 8])
    hnw_d = dram("hnw", [128, DEPTH])
    lbraw_d = dram("lbraw", [128, DEPTH, 8])
    mscan_d = dram("mscan", [128, T])
    mbd_d = dram("mbd", [128, T])
    cb_d = dram("cb", [128, 3, 128])
    band_d = dram("band", [128, 12, 128])
    outT_d = dram("outT", [SEQ_PER_CORE, 128, 8, S], kind="ExternalOutput")
    dbg_d = {}

    def sb(name, shape, dt=F32):
        return stack.enter_context(nc.sbuf_tensor(name, list(shape), dt))

    ps = [stack.enter_context(nc.psum_tensor("ps%d" % i, [128, 512], F32)) for i in range(8)]
    PS = ["ps%d" % i for i in range(8)]

    xg = [sb("xg%d" % i, [128, 8, T]) for i in range(2)]
    hT = sb("hT", [128, 8, T], BF16)
    mixT = sb("mixT", [128, 16, T], BF16)
    sgw = sb("sgw", [128, 8, T], BF16)
    wbuf = [sb("wbuf%d" % i, [128, 8, 512], BF16) for i in range(NWB)]
    woutb = sb("woutb", [128, 16, 1024], BF16)
    poolwb = sb("poolwb", [128, 4, 2, 256], BF16)
    Sst = [sb("Sst%d" % l, [128, 8, 128]) for l in range(DEPTH)]
    uprev = [sb("uprev%d" % l, [128, 4, 256], BF16) for l in range(DEPTH)]
    mscan = sb("mscan_s", [128, T])
    mbd = sb("mbd_s", [128, T])
    cbb = sb("cbb", [128, 3, 128], BF16)
    bandb = sb("bandb", [128, 12, 128], BF16)
    cT = sb("cT_s", [128, 8, SEQ_PER_CORE])
    scT = sb("scT", [128, 8, SEQ_PER_CORE], BF16)
    adab = sb("adab_s", [128, DEPTH, 24])
    modT = sb("modT", [128, DEPTH, 24, SEQ_PER_CORE])
    npre = sb("npre_s", [128, DEPTH, 8])
    pscale = sb("pscale_s", [128, DEPTH, 8])
    npost = sb("npost_s", [128, DEPTH, 8])
    hnw = sb("hnw_s", [128, DEPTH])
    lbraw = sb("lbraw_s", [128, DEPTH, 8])
    lbe = sb("lbe", [128, DEPTH, 8])
    lbs = sb("lbs", [128, 8])
    lbr = sb("lbr", [128, 8])
    lbsm = sb("lbsm", [128, DEPTH, 8])
    lb = sb("lb", [128, DEPTH, 8])
    ln1mlb = sb("ln1mlb", [128, DEPTH, 8])
    Acoef = sb("Acoef", [128, DEPTH, SEQ_PER_CORE, 8])
    SHcoef = sb("SHcoef", [128, DEPTH, SEQ_PER_CORE, 8])
    GWcoef = sb("GWcoef", [128, DEPTH, SEQ_PER_CORE, 8])
    utok = [sb("utok%d" % i, [128, NT, 256], BF16) for i in range(2)]
    dT = [sb("dT%d" % i, [128, 2, T], BF16) for i in range(2)]
    sgp = [sb("sgp%d" % i, [128, 2, T], BF16) for i in range(2)]
    HS = []
    for i in range(2):
        HS.append(dict(
            e_t=sb("e_t%d" % i, [128, T]),
            L1=sb("L1%d" % i, [128, T]),
            L2=sb("L2%d" % i, [128, T]),
            bcum=sb("bcum%d" % i, [128, T]),
            Dm=sb("Dm%d" % i, [128, T]),
            dec=sb("dec%d" % i, [128, NCH]),
            QT=sb("QT%d" % i, [128, T], BF16),
            KT=sb("KT%d" % i, [128, T], BF16),
            vtok=sb("vtok%d" % i, [128, NT, 128], BF16),
            vblk=sb("vblk%d" % i, [128, NT, CPT, 128], BF16),
            ktok=sb("ktok%d" % i, [128, NT, 128], BF16),
            attnm=sb("attnm%d" % i, [128, T], BF16),
            Rbf=sb("Rbf%d" % i, [128, NCH, 128], BF16),
            Sall=sb("Sall%d" % i, [128, 9, 128]),
        ))
    lnv, rstd = HS[0]["bcum"], HS[1]["bcum"]
    tA = [HS[0]["Dm"], HS[1]["Dm"]]
    tO = [HS[0]["L1"], HS[1]["L1"]]

    ident = cbb[:, 0, :]
    if KEEP_WARM:
        P.warm = cbb[:, 0, :]
    onesD = cbb[:, 1, :]
    onesV = cbb[:, 2, :]

    def fsz(ap):
        n = 1
        for d_ in ap.shape[1:]:
            n *= int(d_)
        return n

    def dma(eng, key, out, in_, reads, writes, nbytes=0):
        P.add(eng, lambda e: e.dma_start(out=out, in_=in_), reads, writes, dma=key,
              cost=(1500.0 if eng == "pool" else 300.0), lat=nbytes / 190.0)

    def mm(out, lhsT, rhs, start, stop, reads, writes):
        P.add("pe", lambda e: e.matmul(out, lhsT=lhsT, rhs=rhs, start=start, stop=stop), reads, writes,
              cost=max(fsz(rhs), 64, min(fsz(lhsT), 128) * 0.6) / 1.7 + 8.0, lat=LAT)

    def tr(out, in_, reads, writes):
        P.add("pe", lambda e: e.transpose(out, in_, ident), reads, writes, cost=80.0, lat=LAT)

    def act(out, in_, func, reads, writes, scale=None, bias=None):
        kw = {}
        if scale is not None:
            kw["scale"] = scale
        if bias is not None:
            kw["bias"] = bias
        P.add("act", lambda e: e.activation(out=out, in_=in_, func=func, **kw), reads, writes,
              cost=220.0 + fsz(in_) / 1.3, lat=LAT)

    def tt(out, in0, in1, op, reads, writes, eng="dve"):
        c = 80.0 + fsz(in0) * 1.6 if eng == "dve" else 200.0 + fsz(in0) * 3.0
        P.add(eng, lambda e: e.tensor_tensor(out=out, in0=in0, in1=in1, op=op), reads, writes, cost=c, lat=LAT)

    def ts(out, in0, s1, op0, reads, writes, s2=None, op1=None, eng="dve"):
        c = 80.0 + fsz(in0) * 1.1 if eng == "dve" else 200.0 + fsz(in0) * 2.5
        if op1 is None:
            P.add(eng, lambda e: e.tensor_scalar(out=out, in0=in0, scalar1=s1, scalar2=None, op0=op0), reads, writes,
                  cost=c, lat=LAT)
        else:
            P.add(eng, lambda e: e.tensor_scalar(out=out, in0=in0, scalar1=s1, scalar2=s2, op0=op0, op1=op1),
                  reads, writes, cost=c, lat=LAT)

    def stt(out, in0, scalar, in1, op0, op1, reads, writes):
        P.add("dve", lambda e: e.scalar_tensor_tensor(out=out, in0=in0, scalar=scalar, in1=in1, op0=op0, op1=op1),
              reads, writes, cost=120.0 + fsz(in0) * 1.6, lat=LAT)

    def cp(eng, out, in_, reads, writes):
        c = 80.0 + fsz(in_) * 1.1 if eng == "dve" else 200.0 + fsz(in_) * 2.5
        P.add(eng, lambda e: e.tensor_copy(out=out, in_=in_), reads, writes, cost=c, lat=LAT)

    dma("sp", "c0", cT[:], cT_d, [], ["cT"])
    dma("sp", "c1", adab[:], adab_d, [], ["adab"])
    dma("sp", "c2", npre[:], npre_d, [], ["npre"])
    dma("sp", "c3", pscale[:], pscale_d, [], ["pscale"])
    dma("sp", "c4", npost[:], npost_d, [], ["npost"])
    dma("sp", "c5", hnw[:], hnw_d, [], ["hnw"])
    dma("sp", "c6", lbraw[:], lbraw_d, [], ["lbraw"])
    dma("sp", "c7", mscan[:], mscan_d, [], ["mscan"])
    dma("sp", "c8", mbd[:], mbd_d, [], ["mbd"])
    dma("pool", "c9", cbb[:], cb_d, [], ["cbb"])
    dma("pool", "c10", bandb[:], band_d, [], ["bandb"])

    wcount = [0]

    def load_w(src, ncols):
        i = wcount[0] % NWB
        wcount[0] += 1
        dma("pool", "w%d" % i, wbuf[i][:, :, 0:ncols], src, [], ["wbuf%d" % i], nbytes=128 * 8 * ncols * 4)
        return wbuf[i], "wbuf%d" % i

    for i_ in range(2):
        P.add("dve", (lambda i_=i_: lambda e: e.memset(HS[i_]["vblk"][:], 0.0))(), [], ["vblk%d" % i_])
    act(scT[:], cT[:], AF.Silu, ["cT"], ["scT"])
    def ada_layer(l):
        for blk in range(6):
            wb, wn = load_w(adaw_d[l, blk], 512)
            for j in range(4):
                col = blk * 4 + j
                o = ps[0][:, (l * 24 + col) * SEQ_PER_CORE:(l * 24 + col + 1) * SEQ_PER_CORE]
                for kc in range(8):
                    mm(o, wb[:, kc, j * 128:(j + 1) * 128], scT[:, kc, :], kc == 0, kc == 7,
                       [wn, "scT"], [PS[0]])
        tt(modT[:, l], ps[0][:, l * 24 * SEQ_PER_CORE:(l + 1) * 24 * SEQ_PER_CORE].rearrange(
            "p (c b) -> p c b", b=SEQ_PER_CORE),
           adab[:, l, :].unsqueeze(2).to_broadcast([128, 24, SEQ_PER_CORE]), ALU.add,
           [PS[0], "adab"], ["modT%d" % l])
        for b in range(SEQ_PER_CORE):
            stt(Acoef[:, l, b, :], modT[:, l, 8:16, b], 1.0, npre[:, l, :], ALU.add, ALU.mult,
                ["modT%d" % l, "npre"], ["Acoef%d" % l])
            P.add("dve", (lambda l=l, b=b: lambda e: e.tensor_copy(out=SHcoef[:, l, b, :], in_=modT[:, l, 0:8, b]))(),
                  ["modT%d" % l], ["SHcoef%d" % l])
            tt(GWcoef[:, l, b, :], modT[:, l, 16:24, b], npost[:, l, :], ALU.mult, ["modT%d" % l, "npost"],
               ["GWcoef%d" % l])

    ada_layer(0)
    act(lbe[:], lbraw[:], AF.Exp, ["lbraw"], ["lbe"])
    tt(lbs[:], lbe[:, 0, :], lbe[:, 1, :], ALU.add, ["lbe"], ["lbs"])
    P.add("dve", lambda e: e.reciprocal(out=lbr[:], in_=lbs[:]), ["lbs"], ["lbr"])
    for l in range(DEPTH):
        tt(lbsm[:, l, :], lbe[:, l, :], lbr[:], ALU.mult, ["lbe", "lbr"], ["lbsm"])
    tt(lb[:, 0, :], lbsm[:, 0, :], lbsm[:, 0, :], ALU.subtract, ["lbsm"], ["lb0"])
    tt(lbs[:], lbsm[:, 0, :], lbsm[:, 1, :], ALU.add, ["lbsm", "lbr"], ["lbs2"])
    tt(lb[:, 1, :], lbs[:], lbsm[:, 0, :], ALU.subtract, ["lbs2", "lbsm"], ["lb1"])
    act(ln1mlb[:], lb[:], AF.Ln, ["lb0", "lb1"], ["ln1mlb"], scale=-1.0, bias=1.0)
    LBR = ["lb0", "lb1", "ln1mlb"]

    unit = [0]
    marks = []
    P.marks = marks

    def next_set():
        u = unit[0] % 2
        unit[0] += 1
        return u, 4 * u

    def layer_pass(l, b, G, xt, xn):
        first = (G == 0)
        marks.append(('A l%d b%d G%d' % (l, b, G), len(P.ops)))
        dma("pool", "wo", woutb[:], wout_d[l], [], ["woutb"], nbytes=8 << 20)
        dma("pool", "pw", poolwb[:], poolw_d[:, l], [], ["poolwb"], nbytes=512 << 10)
        u, pb = next_set()
        sqA = mixT[:, 0:8, :]
        for k2 in range(4):
            act(mixT[:, 2 * k2:2 * k2 + 2, :], xt[:, 2 * k2:2 * k2 + 2, :], AF.Square, [xn], ["mixT"])
            for kc in (2 * k2, 2 * k2 + 1):
                mm(ps[pb + 3][:], onesD, mixT[:, kc, :], kc == 0, kc == 7, ["mixT", "cbb"], [PS[pb + 3]])
        act(lnv[:], ps[pb + 3][:], AF.Ln, [PS[pb + 3]], ["bcum0"], bias=EPS)
        act(rstd[:], lnv[:], AF.Exp, ["bcum0"], ["bcum1"], scale=-0.5)
        for kc in range(8):
            tb = tA[kc % 2]
            tn = "Dm%d" % (kc % 2)
            tt(tb[:], xt[:, kc, :], rstd[:], ALU.mult, [xn, "bcum1"], [tn], eng="dve")
            act(hT[:, kc, :], tb[:], AF.Identity, [tn, "Acoef%d" % l, "SHcoef%d" % l], ["hT"],
                scale=Acoef[:, l, b, kc:kc + 1], bias=SHcoef[:, l, b, kc:kc + 1])
        if stop == 'A':
            return
        def unit_P(g):
            marks.append(('P%d l%d' % (g, l), len(P.ops)))
            u, pb = next_set()
            wb, wn = load_w(wpg_d[l, g], 512)
            un, dn, sn_ = "utok%d" % u, "dT%d" % u, "sgp%d" % u
            for oc in range(2):
                for kc in range(8):
                    mm(ps[pb + oc][:], wb[:, kc, 256 + oc * 128:256 + (oc + 1) * 128], hT[:, kc, :], kc == 0, kc == 7,
                       [wn, "hT"], [PS[pb + oc]])
            for j in range(NT):
                o = ps[pb + 2 + j // 2][:, (j % 2) * 256:(j % 2 + 1) * 256]
                for kc in range(8):
                    mm(o, hT[:, kc, j * 128:(j + 1) * 128], wb[:, kc, 0:256], kc == 0, kc == 7,
                       [wn, "hT"], [PS[pb + 2 + j // 2]])
            for oc in range(2):
                act(sgp[u][:, oc, :], ps[pb + oc][:], AF.Silu, [PS[pb + oc]], [sn_])
            for jj in range(NT // 2):
                cp("dve", utok[u][:, 2 * jj:2 * jj + 2, :], ps[pb + 2 + jj][:].rearrange("p (j c) -> p j c", c=256),
                   [PS[pb + 2 + jj]], [un])
            for cc in range(2):
                bk = pb + 2 + cc
                for j in range(NT):
                    o = ps[bk][:, j * 128:(j + 1) * 128]
                    if j == 0:
                        if first:
                            mm(o, utok[u][:, 0, cc * 128:(cc + 1) * 128], bandb[:, 8 + g, :], True, True,
                               [un, "bandb"], [PS[bk]])
                        else:
                            mm(o, utok[u][:, 0, cc * 128:(cc + 1) * 128], bandb[:, g, :], True, False,
                               [un, "bandb"], [PS[bk]])
                            mm(o, uprev[l][:, g, cc * 128:(cc + 1) * 128], bandb[:, 4 + g, :], False, True,
                               ["uprev%d_%d" % (l, g), "bandb"], [PS[bk]])
                    else:
                        mm(o, utok[u][:, j, cc * 128:(cc + 1) * 128], bandb[:, g, :], True, False,
                           [un, "bandb"], [PS[bk]])
                        mm(o, utok[u][:, j - 1, cc * 128:(cc + 1) * 128], bandb[:, 4 + g, :], False, True,
                           [un, "bandb"], [PS[bk]])
            cp("pool", uprev[l][:, g, :], utok[u][:, NT - 1, :], [un], ["uprev%d_%d" % (l, g)])
            for cc in range(2):
                act(dT[u][:, cc, :], ps[pb + 2 + cc][:], AF.Copy, [PS[pb + 2 + cc]], [dn])
            for oc in range(2):
                for cc in range(2):
                    mm(ps[pb + oc][:], poolwb[:, g, cc, oc * 128:(oc + 1) * 128], dT[u][:, cc, :], cc == 0, cc == 1,
                       ["poolwb", dn], [PS[pb + oc]])
            for oc in range(2):
                stt(mixT[:, 2 * g + oc, :], ps[pb + oc][:], pscale[:, l, 2 * g + oc:2 * g + oc + 1], sgp[u][:, oc, :],
                    ALU.mult, ALU.mult, [PS[pb + oc], "pscale", sn_], ["mixT"])
        def unit_G(hb):
            marks.append(('G%d l%d' % (hb, l), len(P.ops)))
            u, pb = next_set()
            wb, wn = load_w(wpg_d[l, 4 + hb], 512)
            for oc in range(4):
                for kc in range(8):
                    mm(ps[pb + oc][:], wb[:, kc, oc * 128:(oc + 1) * 128], hT[:, kc, :], kc == 0, kc == 7,
                       [wn, "hT"], [PS[pb + oc]])
            for oc in range(4):
                act(sgw[:, hb * 4 + oc, :], ps[pb + oc][:], AF.Silu, [PS[pb + oc]], ["sgw%d" % (hb * 4 + oc)])
        def unit_H(h):
            marks.append(('H%d l%d' % (h, l), len(P.ops)))
            u, pb = next_set()
            pq, pf, pv, pa = pb, pb + 1, pb + 2, pb + 3
            X = HS[u]
            e_t, L1, L2, bcum, Dm, dec = X["e_t"], X["L1"], X["L2"], X["bcum"], X["Dm"], X["dec"]
            QT, KT, vtok, vblk, ktok, attnm, Rbf, SA, sqH = (X["QT"], X["KT"], X["vtok"], X["vblk"], X["ktok"],
                                                           X["attnm"], X["Rbf"], X["Sall"], X["attnm"])

            def R(nm, u=u):
                return "%s%d" % (nm, u)

            wb, wn = load_w(whq_d[l, h], 384)
            for kc in range(8):
                mm(ps[pq][:], wb[:, kc, 0:128], hT[:, kc, :], kc == 0, kc == 7, [wn, "hT"], [PS[pq]])
            for kc in range(8):
                mm(ps[pf][:], wb[:, kc, 128:256], hT[:, kc, :], kc == 0, kc == 7, [wn, "hT"], [PS[pf]])
            for j in range(NT):
                for kc in range(8):
                    mm(ps[pv][:, j * 128:(j + 1) * 128], hT[:, kc, j * 128:(j + 1) * 128], wb[:, kc, 256:384],
                       kc == 0, kc == 7, [wn, "hT"], [PS[pv]])
            act(e_t[:], ps[pf][:], AF.Exp, [PS[pf]], [R("e_t")], scale=-1.0)
            act(L2[:], e_t[:], AF.Ln, [R("e_t")], [R("L2")], bias=1.0)
            act(L1[:], e_t[:], AF.Ln, [R("e_t")] + LBR, [R("L1")], scale=lb[:, l, h:h + 1], bias=1.0)
            act(vtok[:], ps[pv][:].rearrange("p (j c) -> p j c", c=128), AF.Copy, [PS[pv]], [R("vtok")])
            for q in range(CPT):
                if q % 2 == 0:
                    act(vblk[q * CH:(q + 1) * CH, :, q, :], ps[pv][q * CH:(q + 1) * CH, :].rearrange("p (j c) -> p j c", c=128),
                        AF.Copy, [PS[pv]], [R("vblk")])
                else:
                    cp("dve", vblk[q * CH:(q + 1) * CH, :, q, :], ps[pv][q * CH:(q + 1) * CH, :].rearrange("p (j c) -> p j c", c=128),
                       [PS[pv]], [R("vblk")])
            tt(L1[:], L1[:], L2[:], ALU.subtract, [R("L1"), R("L2")], [R("L1")], eng=ENG_LF)
            P.add("dve", (lambda bcum=bcum, L1=L1: lambda e: e.tensor_tensor_scan(
                out=bcum[:], data0=mscan[:], data1=L1[:], initial=0.0, op0=ALU.mult, op1=ALU.add))(),
                [R("L1"), "mscan"], [R("bcum")], cost=80.0 + T * 2.1, lat=LAT)
            b3 = bcum[:].rearrange("p (c t) -> p c t", t=CH)
            tt(Dm[:].rearrange("p (c t) -> p c t", t=CH), b3, b3[:, :, CH - 1:CH].to_broadcast([128, NCH, CH]),
               ALU.subtract, [R("bcum")], [R("Dm")])
            act(dec[:], b3[:, :, CH - 1], AF.Exp, [R("bcum")], [R("dec")])
            act(e_t[:], Dm[:], AF.Exp, [R("Dm")], [R("e_t")])
            stt(QT[:], e_t[:], ACLAMP, ps[pq][:], ALU.min, ALU.mult, [PS[pq], R("e_t")], [R("QT")])
            tt(L2[:], ps[pf][:], L2[:], ALU.add, [PS[pf], R("L2")], [R("L2")])
            tt(L2[:], L2[:], Dm[:], ALU.add, [R("L2"), R("Dm")], [R("L2")], eng=ENG_T1D)
            act(KT[:], L2[:], AF.Exp, [R("L2")] + LBR, [R("KT")], scale=-1.0, bias=ln1mlb[:, l, h:h + 1])
            ktp = ps[pv][:].bitcast(BF16)
            for j in range(NT):
                tr(ktp[:, j * 128:(j + 1) * 128], KT[:, j * 128:(j + 1) * 128], [R("KT"), "cbb"], [PS[pv]])
            act(ktok[:].rearrange("p j c -> p (j c)"), ktp[:, 0:T], AF.Copy, [PS[pv]], [R("ktok")])
            for j in range(NT):
                mm(ps[pa][:, j * 128:(j + 1) * 128], KT[:, j * 128:(j + 1) * 128], QT[:, j * 128:(j + 1) * 128],
                   True, True, [R("KT"), R("QT")], [PS[pa]])
            tt(attnm[:], ps[pa][:], mbd[:], ALU.mult, [PS[pa], "mbd"], [R("attnm")])
            sn = "S%d_%d" % (l, h)

            def SL(k_):
                return "%s_%d" % (R("Sall"), k_)

            for hh in range(2):
                if hh == 0:
                    cp("pool", SA[:, 0, :], Sst[l][:, h, :], [sn], [SL(0)])
                else:
                    cp("pool", SA[:, 0, :], SA[:, 8, :], [SL(8)], [SL(0)])
                for jj in range(2):
                    j = 2 * hh + jj
                    bank = (pq, pf)[jj]
                    mm(ps[bank][:], ktok[:, j, :], vblk[:, j, :, :].rearrange("p q c -> p (q c)"), True, True,
                       [R("ktok"), R("vblk")], [PS[bank]])
                for i_ in range(8):
                    c = 8 * hh + i_
                    bank = (pq, pf)[i_ // 4]
                    if hh == 1 or ACT_RBF_ALL:
                        act(Rbf[:, c, :], SA[:, i_, :], AF.Identity, [SL(i_), R("dec")], [R("Rbf") + "_%d" % hh],
                            scale=dec[:, c:c + 1])
                    stt(SA[:, i_ + 1, :], SA[:, i_, :], dec[:, c:c + 1], ps[bank][:, (i_ % 4) * 128:(i_ % 4 + 1) * 128],
                        ALU.mult, ALU.add, [SL(i_), R("dec"), PS[bank]], [SL(i_ + 1)])
                if hh == 0 and not ACT_RBF_ALL:
                    tt(Rbf[:, 0:8, :], SA[:, 0:8, :],
                       dec[:, 0:8].unsqueeze(2).to_broadcast([128, 8, 128]), ALU.mult,
                       [SL(k_) for k_ in range(8)] + [R("dec")], [R("Rbf") + "_0"], eng="pool")
            cp("pool", Sst[l][:, h, :], SA[:, 8, :], [SL(8)], [sn])
            for j in range(NT):
                mm(ps[pa][:, j * 128:(j + 1) * 128], vtok[:, j, :], attnm[:, j * 128:(j + 1) * 128], True, False,
                   [R("vtok"), R("attnm")], [PS[pa]])
                for c in range(CPT * j, CPT * (j + 1)):
                    mm(ps[pa][:, c * CH:(c + 1) * CH], Rbf[:, c, :], QT[:, c * CH:(c + 1) * CH], False,
                       c % CPT == CPT - 1, [R("Rbf") + "_%d" % (c // 8), R("QT")], [PS[pa]])
            act(sqH[:], ps[pa][:], AF.Square, [PS[pa]], [R("attnm")])
            mm(ps[pv][:], onesV, sqH[:], True, True, [R("attnm"), "cbb"], [PS[pv]])
            act(L1[:], ps[pv][:], AF.Ln, [PS[pv]], [R("L1")], bias=EPS)
            act(e_t[:], L1[:], AF.Exp, [R("L1")], [R("e_t")], scale=-0.5)
            tt(L2[:], ps[pa][:], e_t[:], ALU.mult, [PS[pa], R("e_t")], [R("L2")])
            stt(mixT[:, 8 + h, :], L2[:], hnw[:, l:l + 1], sgw[:, h, :], ALU.mult, ALU.mult,
                [R("L2"), "hnw", "sgw%d" % h], ["mixT"])
        seq = ([('P', g) for g in range(4)] + [('G', 0), ('G', 1)] + [('H', h) for h in range(8)]) if not INTERLEAVE \
            else [('G', 0), ('H', 0), ('P', 0), ('H', 1), ('P', 1), ('H', 2), ('G', 1), ('H', 3), ('P', 2),
                  ('H', 4), ('P', 3), ('H', 5), ('H', 6), ('H', 7)]
        for kind, idx in seq:
            {'P': unit_P, 'G': unit_G, 'H': unit_H}[kind](idx)
        for hf in range(T // 256):
            marks.append(('O%d l%d' % (hf, l), len(P.ops)))
            u, pb = next_set()
            pms = 4 * (1 - u) + 3
            tsl = slice(hf * 256, (hf + 1) * 256)
            for oc in range(8):
                o = ps[pb + oc // 2][:, (oc % 2) * 256:(oc % 2 + 1) * 256]
                for kc in range(16):
                    mm(o, woutb[:, kc, oc * 128:(oc + 1) * 128], mixT[:, kc, tsl], kc == 0, kc == 15,
                       ["woutb", "mixT"], [PS[pb + oc // 2]])
            for bk in range(4):
                act(hT[:, 2 * bk:2 * bk + 2, tsl], ps[pb + bk][:].rearrange("p (o t) -> p o t", t=256), AF.Square,
                    [PS[pb + bk]], ["hT"])
            for kc in range(8):
                mm(ps[pms][:, 0:256], onesD, hT[:, kc, tsl], kc == 0, kc == 7, ["hT", "cbb"], [PS[pms]])
            act(lnv[:, 0:256], ps[pms][:, 0:256], AF.Ln, [PS[pms]], ["bcum0"], bias=EPS)
            act(rstd[:, 0:256], lnv[:, 0:256], AF.Exp, ["bcum0"], ["bcum1"], scale=-0.5)
            for oc in range(8):
                tb = tO[oc % 2]
                tn = "L1%d" % (oc % 2)
                stt(tb[:, 0:256], ps[pb + oc // 2][:, (oc % 2) * 256:(oc % 2 + 1) * 256], GWcoef[:, l, b, oc:oc + 1],
                    rstd[:, 0:256], ALU.mult, ALU.mult, [PS[pb + oc // 2], "GWcoef%d" % l, "bcum1"], [tn])
                tt(xt[:, oc, tsl], xt[:, oc, tsl], tb[:, 0:256], ALU.add, [xn, tn], [xn],
                   eng="dve")

    out_keys = []
    it = 0
    for b in range(n_seq):
        for l in range(depth):
            P.add("dve", (lambda l=l: lambda e: e.memset(Sst[l][:], 0.0))(), [],
                  ["S%d_%d" % (l, h) for h in range(8)])
        for G in range(n_groups):
            xt = xg[it % 2]
            xn = "xg%d" % (it % 2)
            dma("sp", "x%d" % (it % 2), xt[:], xT_d[b, :, :, G * T:(G + 1) * T], [], [xn], nbytes=2 << 20)
            for l in range(depth):
                layer_pass(l, b, G, xt, xn)
                if it == 0 and l == 0 and depth > 1:
                    ada_layer(1)
            dma("sp", "o%d" % (it % 2), outT_d[b, :, :, G * T:(G + 1) * T], xt[:], [xn], ["out%d" % (it % 2)],
                nbytes=2 << 20)
            it += 1
    P.add("sp", None, ["out0", "out1"], [])
    P.emit(nc, stack)
    stack.close()
    return nc


def _prep_shared(norm_pre_w, ada_w, ada_b, w_in, pool_w, pool_scale, hgrn_lower_bounds, hgrn_norm_w, w_out,
                 norm_post_w):
    f = np.float32
    ada_w = np.asarray(ada_w, f)
    w_in = np.asarray(w_in, f)

    def rows(w):
        L, R, C = w.shape
        return w.reshape(L, R // 128, 128, C).transpose(0, 2, 1, 3)

    adaw = rows(ada_w).reshape(DEPTH, 128, 8, 6, 512).transpose(0, 3, 1, 2, 4)
    win = rows(w_in)
    blocks = []
    for g in range(4):
        blocks.append(np.concatenate([win[..., 256 * g:256 * (g + 1)], win[..., 1024 + 256 * g:1024 + 256 * (g + 1)]],
                                     axis=-1))
    for hb in range(2):
        blocks.append(win[..., 5120 + 512 * hb:5120 + 512 * (hb + 1)])
    wpg = np.stack(blocks, axis=1)
    hq = []
    for h in range(8):
        hq.append(np.concatenate([win[..., 2048 + 128 * h:2048 + 128 * (h + 1)],
                                  win[..., 3072 + 128 * h:3072 + 128 * (h + 1)],
                                  win[..., 4096 + 128 * h:4096 + 128 * (h + 1)]], axis=-1))
    whq = np.stack(hq, axis=1)
    poolw = np.asarray(pool_w, f).reshape(DEPTH, 4, 2, 128, 256).transpose(3, 0, 1, 2, 4)
    wout = np.asarray(w_out, f).reshape(DEPTH, 16, 128, 1024).transpose(0, 2, 1, 3)

    def vec8(v):
        return np.asarray(v, f).reshape(DEPTH, 8, 128).transpose(2, 0, 1)

    mask_scan, mask_bd, cb, band = _consts()
    d = dict(
        adaw=adaw, adab=np.asarray(ada_b, f).reshape(DEPTH, 24, 128).transpose(2, 0, 1),
        wpg=wpg, whq=whq, poolw=poolw, wout=wout,
        npre=vec8(norm_pre_w), pscale=vec8(pool_scale), npost=vec8(norm_post_w),
        hnw=np.asarray(hgrn_norm_w, f).T, lbraw=vec8(hgrn_lower_bounds),
        mscan=mask_scan, mbd=mask_bd, cb=cb, band=band,
    )
    return {k: np.ascontiguousarray(v, dtype=f) for k, v in d.items()}


def kernel(x, c, norm_pre_w, ada_w, ada_b, w_in, pool_w, pool_scale, hgrn_lower_bounds, hgrn_norm_w, w_out,
           norm_post_w):
    x = np.asarray(x, np.float32)
    c = np.asarray(c, np.float32)
    shared = _prep_shared(norm_pre_w, ada_w, ada_b, w_in, pool_w, pool_scale, hgrn_lower_bounds, hgrn_norm_w,
                          w_out, norm_post_w)
    in_maps = []
    for i in range(NCORES):
        xs = x[i * SEQ_PER_CORE:(i + 1) * SEQ_PER_CORE]
        xT = np.ascontiguousarray(xs.reshape(SEQ_PER_CORE, S, 8, 128).transpose(0, 3, 2, 1))
        cs = c[i * SEQ_PER_CORE:(i + 1) * SEQ_PER_CORE]
        cT = np.ascontiguousarray(cs.reshape(SEQ_PER_CORE, 8, 128).transpose(2, 1, 0))
        m = dict(shared)
        m["xT"] = xT
        m["cT"] = cT
        in_maps.append(m)
    nc = build()
    res = run_bass_kernel_spmd(nc, in_maps, core_ids=list(range(NCORES)))
    out = np.empty((B, S, D), np.float32)
    for i in range(NCORES):
        oT = np.asarray(res.results[i]["outT"]).reshape(SEQ_PER_CORE, 128, 8, S)
        out[i * SEQ_PER_CORE:(i + 1) * SEQ_PER_CORE] = oT.transpose(0, 3, 2, 1).reshape(SEQ_PER_CORE, S, D)
    return out
```
